# Optimizing a Trainium2 kernel written in Bass

```python
import math
import jax, jax.numpy as jnp
from jax import lax
import numpy as np

D_MODEL = 1024
BATCH = 16
SEQ = 2048
DEPTH = 1

MLA_HEADS = 8
QK_NOPE = 64
QK_ROPE = 32
V_DIM = 64
Q_LORA = 384
KV_LORA = 256
ROPE_BASE = 10000.0
Q_BLOCK = 128
MLA_OUT = MLA_HEADS * V_DIM
RWKV_HEADS = 8
RWKV_HEAD = 64
RWKV_DIM = RWKV_HEADS * RWKV_HEAD
DECAY_LORA = 64
AAA_LORA = 64
GATE_LORA = 128
LNX_EPS = 64e-5
N_DIR = 2
N_BRANCH = 2
MLA_IN = Q_LORA + KV_LORA + QK_ROPE
RWKV_IN = 3 * RWKV_DIM + N_DIR * DECAY_LORA + N_DIR * AAA_LORA + GATE_LORA
GATE_IN = N_BRANCH * D_MODEL
IN_DIM = MLA_IN + RWKV_IN + GATE_IN
N_GROUPS = 4
EXPERTS_PER_GROUP = 8
N_EXPERTS = N_GROUPS * EXPERTS_PER_GROUP
TOP_K = 2
D_EXPERT = 256
MOE_BLOCK = 128
NORM_EPS = 1e-6

kernel_name = 'hybrid_mla_birwkv7_hiermoe_encoder'


def rms_norm(x, g, eps=NORM_EPS):
    xf = x.astype(jnp.float32)
    y = xf * lax.rsqrt(jnp.mean(xf * xf, axis=-1, keepdims=True) + eps)
    return (y * g.astype(jnp.float32)).astype(x.dtype)


def apply_rope(x, cos, sin):
    half = x.shape[-1] // 2
    x1 = x[..., :half].astype(jnp.float32)
    x2 = x[..., half:].astype(jnp.float32)
    out = jnp.concatenate([x1 * cos - x2 * sin, x1 * sin + x2 * cos], axis=-1)
    return out.astype(x.dtype)


def mla_branch(z, q_norm_g, kv_norm_g, w_uq, w_ukv):
    B, S, _ = z.shape
    c_q = rms_norm(z[..., :Q_LORA], q_norm_g)
    c_kv = rms_norm(z[..., Q_LORA:Q_LORA + KV_LORA], kv_norm_g)
    k_rope = z[..., Q_LORA + KV_LORA:]
    q = (c_q @ w_uq).reshape(B, S, MLA_HEADS, QK_NOPE + QK_ROPE)
    kv = (c_kv @ w_ukv).reshape(B, S, MLA_HEADS, QK_NOPE + V_DIM)
    q_nope, q_rope = q[..., :QK_NOPE], q[..., QK_NOPE:]
    k_nope, v = kv[..., :QK_NOPE], kv[..., QK_NOPE:]
    pos = jnp.arange(S, dtype=jnp.float32)
    inv_freq = ROPE_BASE ** (-jnp.arange(0, QK_ROPE, 2, dtype=jnp.float32) / QK_ROPE)
    ang = pos[:, None] * inv_freq[None, :]
    cos, sin = jnp.cos(ang), jnp.sin(ang)
    q_rope = apply_rope(q_rope, cos[:, None, :], sin[:, None, :])
    k_rope = apply_rope(k_rope, cos, sin)
    scale = (QK_NOPE + QK_ROPE) ** -0.5
    nb = S // Q_BLOCK
    qn_b = q_nope.reshape(B, nb, Q_BLOCK, MLA_HEADS, QK_NOPE).transpose(1, 0, 2, 3, 4)
    qr_b = q_rope.reshape(B, nb, Q_BLOCK, MLA_HEADS, QK_ROPE).transpose(1, 0, 2, 3, 4)

    def attend(blk):
        qn, qr = blk
        s = (jnp.einsum('bqhd,bkhd->bhqk', qn, k_nope)
             + jnp.einsum('bqhd,bkd->bhqk', qr, k_rope))
        p = jax.nn.softmax(s.astype(jnp.float32) * scale, axis=-1).astype(v.dtype)
        return jnp.einsum('bhqk,bkhd->bqhd', p, v)

    o = lax.map(attend, (qn_b, qr_b))
    return o.transpose(1, 0, 2, 3, 4).reshape(B, S, MLA_OUT)


def token_shift_centred(z, mu_prev, mu_next):
    z_prev = jnp.pad(z[:, :-1], ((0, 0), (1, 0), (0, 0)))
    z_next = jnp.pad(z[:, 1:], ((0, 0), (0, 1), (0, 0)))
    return z + mu_prev * (z_prev - z) + mu_next * (z_next - z)


def wkv7_scan(r, w, k, v, a_vec, b_vec):
    B, S, H, N = r.shape

    def step(state, inp):
        r_t, w_t, k_t, v_t, a_t, b_t = inp
        sa = jnp.einsum('bhvk,bhk->bhv', state, a_t)
        state = (state * w_t[:, :, None, :] + sa[..., None] * b_t[:, :, None, :]
                 + v_t[..., None] * k_t[:, :, None, :])
        return state, jnp.einsum('bhvk,bhk->bhv', state, r_t)

    xs = tuple(jnp.moveaxis(t, 1, 0) for t in (r, w, k, v, a_vec, b_vec))
    s0 = jnp.zeros((B, H, N, N), jnp.float32)
    _, y = lax.scan(step, s0, xs)
    return jnp.moveaxis(y, 0, 1)


def rwkv_branch(z, mu_prev, mu_next, w0, w_up, a0, a_up, g_up, k_k, k_a, r_k, lnx_g, lnx_b):
    B, S, _ = z.shape
    C, H, N = RWKV_DIM, RWKV_HEADS, RWKV_HEAD
    f32 = jnp.float32
    zf = token_shift_centred(z.astype(f32), mu_prev.astype(f32), mu_next.astype(f32))
    r, k, v = zf[..., :C], zf[..., C:2 * C], zf[..., 2 * C:3 * C]
    o = 3 * C
    wd = zf[..., o:o + N_DIR * DECAY_LORA].reshape(B, S, N_DIR, DECAY_LORA)
    o += N_DIR * DECAY_LORA
    ad = zf[..., o:o + N_DIR * AAA_LORA].reshape(B, S, N_DIR, AAA_LORA)
    o += N_DIR * AAA_LORA
    gd = zf[..., o:]
    w_logit = w0.astype(f32) + jnp.einsum('bsdr,drc->bsdc', jnp.tanh(wd), w_up.astype(f32))
    decay = jnp.exp(-jnp.exp(-jax.nn.softplus(-w_logit) - 0.5))
    a = jax.nn.sigmoid(a0.astype(f32) + jnp.einsum('bsdr,drc->bsdc', ad, a_up.astype(f32)))
    g = jax.nn.sigmoid(gd) @ g_up.astype(f32)

    def heads(t):
        return t.reshape(t.shape[:-1] + (H, N))

    kk = heads(k * k_k.astype(f32))
    kk = kk / jnp.maximum(jnp.sqrt(jnp.sum(kk * kk, axis=-1, keepdims=True)), 1e-12)
    k_dir = heads(k[:, :, None, :] * (1.0 + (a - 1.0) * k_a.astype(f32)))
    a_h, decay_h = heads(a), heads(decay)
    r_h, v_h = heads(r), heads(v)
    y_f = wkv7_scan(r_h, decay_h[:, :, 0], k_dir[:, :, 0], v_h, -kk, kk * a_h[:, :, 0])
    fl = lambda t: jnp.flip(t, axis=1)
    y_b = fl(wkv7_scan(fl(r_h), fl(decay_h[:, :, 1]), fl(k_dir[:, :, 1]), fl(v_h),
                       fl(-kk), fl(kk * a_h[:, :, 1])))
    y = y_f + y_b
    mu = jnp.mean(y, axis=-1, keepdims=True)
    var = jnp.mean(jnp.square(y - mu), axis=-1, keepdims=True)
    y = ((y - mu) * lax.rsqrt(var + LNX_EPS)).reshape(B, S, C)
    y = y * lnx_g.astype(f32) + lnx_b.astype(f32)
    bonus = jnp.sum(r_h[:, :, None] * k_dir * heads(r_k.astype(f32)), axis=-1, keepdims=True) * v_h[:, :, None]
    y = y + jnp.sum(bonus, axis=2).reshape(B, S, C)
    return (y * g).astype(z.dtype)


def hier_moe(h, rg_w, rg_b, re_w, re_b, w1, w3, w2):
    B, S, D = h.shape
    T = B * S
    ht = h.reshape(T, D)
    group_p = jax.nn.softmax((ht @ rg_w + rg_b).astype(jnp.float32), axis=-1)
    gp, gi = lax.top_k(group_p, 1)
    gp, gi = gp[:, 0], gi[:, 0]
    e_logits = (ht @ re_w + re_b).astype(jnp.float32).reshape(T, N_GROUPS, EXPERTS_PER_GROUP)
    sel = jnp.take_along_axis(e_logits, gi[:, None, None], axis=1)[:, 0]
    ep = jax.nn.softmax(sel, axis=-1)
    tp, ti = lax.top_k(ep, TOP_K)
    tp = tp / jnp.sum(tp, axis=-1, keepdims=True)
    weight = gp[:, None] * tp
    expert = gi[:, None] * EXPERTS_PER_GROUP + ti
    A = T * TOP_K
    flat_e = expert.reshape(A)
    flat_w = weight.reshape(A)
    flat_tok = jnp.repeat(jnp.arange(T, dtype=jnp.int32), TOP_K)
    order = jnp.argsort(flat_e)
    se, stok, sw = flat_e[order], flat_tok[order], flat_w[order]
    counts = jnp.bincount(flat_e, length=N_EXPERTS)
    padded = ((counts + MOE_BLOCK - 1) // MOE_BLOCK) * MOE_BLOCK
    start = jnp.cumsum(counts) - counts
    pend = jnp.cumsum(padded)
    pstart = pend - padded
    dest = pstart[se] + (jnp.arange(A, dtype=jnp.int32) - start[se])
    n_blocks = -(-A // MOE_BLOCK) + N_EXPERTS
    cap = n_blocks * MOE_BLOCK
    slot_tok = jnp.full((cap,), T, jnp.int32).at[dest].set(stok)
    slot_w = jnp.zeros((cap,), h.dtype).at[dest].set(sw.astype(h.dtype))
    block_e = jnp.minimum(jnp.searchsorted(pend, jnp.arange(n_blocks) * MOE_BLOCK, side='right'),
                          N_EXPERTS - 1)
    h_pad = jnp.concatenate([ht, jnp.zeros((1, D), ht.dtype)], axis=0)
    xb = h_pad[slot_tok].reshape(n_blocks, MOE_BLOCK, D)

    def expert_block(args):
        xblk, e = args
        hid = jax.nn.silu(xblk @ w1[e]) * (xblk @ w3[e])
        return hid @ w2[e]

    yb = lax.map(expert_block, (xb, block_e)).reshape(cap, D)
    y = jax.ops.segment_sum(yb * slot_w[:, None], slot_tok, num_segments=T + 1)[:T]
    return y.reshape(B, S, D)


def setup_inputs(seed: int = 0) -> dict:
    key = jax.random.key(seed)
    ks = iter(jax.random.split(key, 48))
    L, D, C = DEPTH, D_MODEL, RWKV_DIM
    f32 = jnp.float32
    nrm = lambda shape, s: jax.random.normal(next(ks), shape, f32) * s
    uni = lambda shape, lo, hi: jax.random.uniform(next(ks), shape, f32, lo, hi)
    return {
        'x': nrm((BATCH, SEQ, D), 1.0),
        'norm_mix_g': 1.0 + nrm((L, D), 0.02),
        'w_in': nrm((L, D, IN_DIM), D ** -0.5),
        'q_norm_g': 1.0 + nrm((L, Q_LORA), 0.02),
        'kv_norm_g': 1.0 + nrm((L, KV_LORA), 0.02),
        'w_uq': nrm((L, Q_LORA, MLA_HEADS * (QK_NOPE + QK_ROPE)), Q_LORA ** -0.5),
        'w_ukv': nrm((L, KV_LORA, MLA_HEADS * (QK_NOPE + V_DIM)), KV_LORA ** -0.5),
        'w_br_mla': nrm((L, MLA_OUT, D), MLA_OUT ** -0.5),
        'mu_prev': uni((L, RWKV_IN), 0.0, 0.5),
        'mu_next': uni((L, RWKV_IN), 0.0, 0.5),
        'w0': uni((L, N_DIR, C), -3.0, 1.0),
        'w_up': nrm((L, N_DIR, DECAY_LORA, C), 0.5 * DECAY_LORA ** -0.5),
        'a0': nrm((L, N_DIR, C), 0.1),
        'a_up': nrm((L, N_DIR, AAA_LORA, C), 0.5 * AAA_LORA ** -0.5),
        'g_up': nrm((L, GATE_LORA, C), GATE_LORA ** -0.5),
        'k_k': 0.85 + nrm((L, C), 0.02),
        'k_a': 1.0 + nrm((L, C), 0.02),
        'r_k': nrm((L, C), 0.1),
        'lnx_g': 1.0 + nrm((L, C), 0.02),
        'lnx_b': nrm((L, C), 0.01),
        'w_br_rwkv': nrm((L, C, D), C ** -0.5),
        'gate_b': nrm((L, N_BRANCH, D), 0.01),
        'w_out': nrm((L, D, D), D ** -0.5),
        'norm_ffn_g': 1.0 + nrm((L, D), 0.02),
        'router_group_w': nrm((L, D, N_GROUPS), D ** -0.5),
        'router_group_b': nrm((L, N_GROUPS), 0.01),
        'router_expert_w': nrm((L, D, N_EXPERTS), D ** -0.5),
        'router_expert_b': nrm((L, N_EXPERTS), 0.01),
        'w1': nrm((L, N_EXPERTS, D, D_EXPERT), D ** -0.5),
        'w3': nrm((L, N_EXPERTS, D, D_EXPERT), D ** -0.5),
        'w2': nrm((L, N_EXPERTS, D_EXPERT, D), D_EXPERT ** -0.5),
        'norm_final_g': 1.0 + nrm((D,), 0.02),
    }


def reference(x, norm_mix_g, w_in, q_norm_g, kv_norm_g, w_uq, w_ukv, w_br_mla,
              mu_prev, mu_next, w0, w_up, a0, a_up, g_up, k_k, k_a, r_k, lnx_g, lnx_b,
              w_br_rwkv, gate_b, w_out, norm_ffn_g, router_group_w, router_group_b,
              router_expert_w, router_expert_b, w1, w3, w2, norm_final_g):
    B, S, D = x.shape
    for l in range(DEPTH):
        h = rms_norm(x, norm_mix_g[l])
        z = h @ w_in[l]
        z_mla = z[..., :MLA_IN]
        z_rwkv = z[..., MLA_IN:MLA_IN + RWKV_IN]
        z_gate = z[..., MLA_IN + RWKV_IN:].reshape(B, S, N_BRANCH, D)
        o_mla = mla_branch(z_mla, q_norm_g[l], kv_norm_g[l], w_uq[l], w_ukv[l])
        o_rwkv = rwkv_branch(z_rwkv, mu_prev[l], mu_next[l], w0[l], w_up[l], a0[l], a_up[l],
                             g_up[l], k_k[l], k_a[l], r_k[l], lnx_g[l], lnx_b[l])
        gates = jax.nn.sigmoid(z_gate + gate_b[l])
        mixed = gates[:, :, 0] * (o_mla @ w_br_mla[l]) + gates[:, :, 1] * (o_rwkv @ w_br_rwkv[l])
        x = x + mixed @ w_out[l]
        h2 = rms_norm(x, norm_ffn_g[l])
        x = x + hier_moe(h2, router_group_w[l], router_group_b[l], router_expert_w[l],
                         router_expert_b[l], w1[l], w3[l], w2[l])
    return rms_norm(x, norm_final_g)
```

```python
import numpy as np
import concourse.bass as bass
import concourse.mybir as mybir
from contextlib import ExitStack

F32 = mybir.dt.float32
BF16 = mybir.dt.bfloat16
I32 = mybir.dt.int32
AF = mybir.ActivationFunctionType
ALU = mybir.AluOpType
AX = mybir.AxisListType

ENGS = ['pe', 'act', 'dve', 'pool', 'sp']
CENGS = ['pe', 'act', 'dve', 'pool']
WINDOW = 10 ** 9


class Prog:
    def __init__(self, nc, stack):
        self.nc = nc
        self.stack = stack
        self.ops = {e: [] for e in ENGS}
        self.count = {e: 0 for e in CENGS}
        self.known = {e: {f: 0 for f in CENGS} for e in ENGS}
        self.dknown = {e: {} for e in ENGS}
        self.clock = {e: [None] for e in CENGS}
        self.sem = {e: stack.enter_context(nc.semaphore('s_' + e)) for e in CENGS}
        self.NDMA = 16
        self.dsem = {q: [stack.enter_context(nc.semaphore('d_%s%d' % (q, i))) for i in range(self.NDMA)]
                     for q in ['sp', 'pool']}
        self.dcount = {'sp': 0, 'pool': 0}
        self.lastw = {}
        self.readers = {}
        self.nwaits = 0

    def _merge(self, E, kn, dk):
        k = self.known[E]
        for f, v in kn.items():
            if v > k[f]:
                k[f] = v
        d = self.dknown[E]
        for f, v in dk.items():
            if v > d.get(f, 0):
                d[f] = v

    def _need(self, E, ev, waits):
        if ev[0] == 'c':
            _, F, n = ev
            if F == E:
                if E == 'pe':
                    return
                if n <= self.known[E][E] or n < self.count[E] - WINDOW + 1:
                    return
            elif self.known[E][F] >= n:
                return
            waits[('c', F)] = max(waits.get(('c', F), 0), n)
            kn, dk = self.clock[F][n]
            self._merge(E, kn, dk)
        else:
            _, q, k, target, kn, dk = ev
            if self.dknown[E].get((q, k), 0) >= target:
                return
            waits[('d', q, k)] = max(waits.get(('d', q, k), 0), target)
            self.dknown[E][(q, k)] = target
            self._merge(E, kn, dk)

    def add(self, E, fn, r=(), w=(), dma=False):
        waits = {}
        deps = []
        for res in r:
            ev = self.lastw.get(res)
            if ev is not None:
                deps.append(ev)
        for res in w:
            ev = self.lastw.get(res)
            if ev is not None:
                deps.append(ev)
            rd = self.readers.get(res)
            if rd:
                deps.extend(rd.values())
        if dma:
            j = self.dcount[E]
            k = j % self.NDMA
            target = 16 * (j // self.NDMA + 1)
            if j >= self.NDMA:
                waits[('d', E, k)] = target - 16
                self.dknown[E][(E, k)] = target - 16
            self.dcount[E] += 1
        for ev in deps:
            self._need(E, ev, waits)
        wl = []
        for key, v in waits.items():
            if key[0] == 'c':
                wl.append((self.sem[key[1]], v))
            else:
                wl.append((self.dsem[key[1]][key[2]], v))
        self.nwaits += len(wl)
        if dma:
            myev = ('d', E, k, target, dict(self.known[E]), dict(self.dknown[E]))
            self.ops[E].append((wl, fn, self.dsem[E][k], 16))
            rkey = ('d', E, k)
        else:
            self.count[E] += 1
            n = self.count[E]
            kn = dict(self.known[E])
            kn[E] = n
            self.clock[E].append((kn, dict(self.dknown[E])))
            myev = ('c', E, n)
            self.ops[E].append((wl, fn, self.sem[E], 1))
            rkey = ('c', E)
        for res in r:
            self.readers.setdefault(res, {})[rkey] = myev
        for res in w:
            self.lastw[res] = myev
            self.readers[res] = {}
        return myev

    def pe(self, fn, r=(), w=()):
        return self.add('pe', fn, r, w)

    def act(self, fn, r=(), w=()):
        return self.add('act', fn, r, w)

    def dve(self, fn, r=(), w=()):
        return self.add('dve', fn, r, w)

    def pool(self, fn, r=(), w=()):
        return self.add('pool', fn, r, w)

    def dma(self, fn, r=(), w=(), q='sp'):
        return self.add(q, fn, r, w, dma=True)

    def barrier(self):
        for E in ENGS:
            wl = []
            for F in CENGS:
                n = self.count[F]
                if n > self.known[E][F] and n > 0:
                    wl.append((self.sem[F], n))
                    self.known[E][F] = n
            for q in ['sp', 'pool']:
                j = self.dcount[q]
                for k in range(min(j, self.NDMA)):
                    cnt = (j - 1 - k) // self.NDMA + 1
                    t = 16 * cnt
                    if self.dknown[E].get((q, k), 0) < t:
                        wl.append((self.dsem[q][k], t))
                        self.dknown[E][(q, k)] = t
            self.ops[E].append((wl, None, None, 0))
            self.nwaits += len(wl)
        self.lastw = {}
        self.readers = {}

    def emit(self):
        nc = self.nc
        self.barrier()
        ops = self.ops

        def run(E, eng):
            for wl, fn, sem, inc in ops[E]:
                for s, v in wl:
                    eng.wait_ge(s, v)
                if fn is not None:
                    fn(eng).then_inc(sem, inc)

        with nc.Block() as block:
            @block.tensor
            def _(e):
                run('pe', e)

            @block.scalar
            def _(e):
                run('act', e)

            @block.vector
            def _(e):
                run('dve', e)

            @block.gpsimd
            def _(e):
                run('pool', e)

            @block.sync
            def _(e):
                run('sp', e)

from concourse.bass_utils import run_bass_kernel_spmd
import ml_dtypes

S = 2048
D = 1024
NW = 4640 + 256
KA_OFF = 4640
KB_OFF = 4768
NSEQ_CORE = 2
EPS = 1e-6
ATT_SCALE = 96 ** -0.5


class T:
    def __init__(self, h, name):
        self.h = h
        self.name = name

    def __getitem__(self, k):
        return self.h[k]


class Ctx:
    pass


def ops_helpers(P):
    H = Ctx()

    def mm(out, lhsT, rhs, start, stop, r, w):
        P.pe(lambda e: e.matmul(out, lhsT, rhs, start=start, stop=stop), r, w)

    def tr(out, in_, ident, r, w):
        P.pe(lambda e: e.transpose(out, in_, ident), r, w)

    def cp(eng, out, in_, r, w):
        if eng == 'act':
            P.act(lambda e: e.copy(out, in_), r, w)
        else:
            P.add(eng, lambda e: e.tensor_copy(out, in_), r, w)

    def tt(eng, out, a, b, op, r, w):
        P.add(eng, lambda e: e.tensor_tensor(out, a, b, op), r, w)

    def ts(eng, out, a, s1, s2, op0, op1, r, w):
        if op1 is None:
            P.add(eng, lambda e: e.tensor_scalar(out, a, s1, None, op0), r, w)
        else:
            P.add(eng, lambda e: e.tensor_scalar(out, a, s1, s2, op0, op1), r, w)

    def stt(eng, out, in0, scalar, in1, op0, op1, r, w):
        eng = 'dve'
        P.add(eng, lambda e: e.scalar_tensor_tensor(out, in0, scalar, in1, op0, op1), r, w)

    def actf(out, in_, func, r, w, bias=None, scale=None):
        kw = {}
        if bias is not None:
            kw['bias'] = bias
        if scale is not None:
            kw['scale'] = scale
        P.act(lambda e: e.activation(out, in_, func, **kw), r, w)

    def red(eng, out, in_, op, r, w):
        P.add(eng, lambda e: e.tensor_reduce(out, in_, AX.X, op), r, w)

    def recip(out, in_, r, w):
        P.dve(lambda e: e.reciprocal(out, in_), r, w)

    def rsqrt(out, in_, scale, bias, r, w):
        actf(out, in_, AF.Sqrt, r, w, bias=bias, scale=scale)
        recip(out, out, w, w)

    H.rsqrt = rsqrt

    def rsqrt_act(out, in_, scale, bias, r, w):
        actf(out, in_, AF.Ln, r, w, bias=bias, scale=scale)
        actf(out, out, AF.Exp, w, w, scale=-0.5)

    H.rsqrt_act = rsqrt_act

    def dma(out, in_, r, w, q='sp'):
        P.dma(lambda e: e.dma_start(out=out, in_=in_), r, w, q=q)

    def memset(eng, ap, val, w):
        P.add(eng, lambda e: e.memset(ap, val), (), w)

    H.mm, H.tr, H.cp, H.tt, H.ts, H.stt, H.actf, H.red, H.recip, H.dma, H.memset = \
        mm, tr, cp, tt, ts, stt, actf, red, recip, dma, memset
    return H


def build_nc(nseq=NSEQ_CORE, phases=('0', 'A', 'C', 'D', 'E', 'F'), dbg=()):
    nc = bass.Bass("TRN2", target_bir_lowering=False)
    G = Ctx()
    G.nc = nc
    G.nseq = nseq
    G.dbg = dbg

    def din(name, shape, dt=F32):
        return nc.dram_tensor(name, list(shape), dt, kind="ExternalInput")

    def dscr(name, shape, dt):
        kind = "ExternalOutput" if name in dbg else "Internal"
        return nc.dram_tensor(name, list(shape), dt, kind=kind)

    I = Ctx()
    G.I = I
    I.x = din('x', [nseq * S, D])
    I.w_in = din('w_in_ext', [D, NW])
    I.g_mix = din('g_mix', [128, 8])
    I.wuq = din('wuq_ext', [384, 2048])
    I.g_q = din('g_q', [128, 3])
    I.wukv = din('wukv_ext', [256, 2048])
    I.g_kv = din('g_kv', [128, 2])
    I.cos128 = din('cos128', [128, S])
    I.sin128 = din('sin128', [128, S])
    I.ident = din('ident_bf', [128, 128], BF16)
    I.ones = din('ones_bf', [128, 128], BF16)
    I.onespad = din('onespad_bf', [2, 128, 128], BF16)
    I.rw_cst = din('rw_cst', [128, 64])
    I.w_up = din('w_up2', [128, 512])
    I.a_up = din('a_up2', [128, 512])
    I.g_up = din('g_up2', [128, 512])
    I.w0b = din('w0b', [2, 128, 512])
    I.tri3 = din('tri3', [2, 128, 384])
    I.maskT = din('maskT', [2, 128, 512], BF16)
    I.maskL = din('maskL', [2, 128, 512], BF16)
    I.ident4 = din('ident4', [128, 512], BF16)
    I.bdones = din('bdones_bf', [128, 128], BF16)
    I.bdones_f = din('bdones_f', [128, 128])
    I.w_br_mla = din('w_br_mla', [512, 1024])
    I.w_br_rwkv = din('w_br_rwkv', [512, 1024])
    I.w_out = din('w_out', [1024, 1024])
    I.gate_b = din('gate_b2', [128, 16])
    I.gffn_b = din('gffn_b', [128, 8, 256])
    I.gffn_row_b = din('gffn_row_b', [128, 1024])
    I.ident_f = din('ident_f', [128, 128])
    I.tris = din('tris_bf', [128, 128], BF16)
    I.rw_router = din('rw_router', [128, 8, 36])
    I.rbias_b = din('rbias_b', [128, 36])
    I.ecap_b = din('ecap_b', [128, 32])
    I.ecap_all = din('ecap_all', [128, 32, 32])
    I.w1 = din('w1', [32, 1024, 256])
    I.w3 = din('w3', [32, 1024, 256])
    I.w2 = din('w2', [32, 256, 1024])
    I.gfin_b = din('gfin_b', [128, 1024])

    Sx = Ctx()
    G.S = Sx
    Sx.Wbf = dscr('Wbf', [8, 128, NW], BF16)
    Sx.hT = dscr('hT_d', [nseq, 8, 128, S], BF16)
    Sx.o1 = dscr('o1_d', [nseq, 4, 128, S], BF16)
    Sx.o2 = dscr('o2_d', [nseq, 4, 128, S], BF16)
    Sx.rw = dscr('rw_d', [nseq, 4, 4, 128, S], BF16)
    Sx.x1 = dscr('x1_d', [nseq * S, D], F32)
    Sx.wbm = dscr('wbm_d', [4, 128, 1024], BF16)
    Sx.wbr = dscr('wbr_d', [4, 128, 1024], BF16)
    Sx.wout = dscr('wout_d', [8, 128, 1024], BF16)
    Sx.xg = dscr('xg_d', [32 * CAP, D], BF16)
    Sx.yg = dscr('yg_d', [32 * CAP, D], BF16)
    G.out = nc.dram_tensor('out', [nseq * S, D], F32, kind="ExternalOutput")

    with ExitStack() as stack:
        P = Prog(nc, stack)
        G.P = P
        G.H = ops_helpers(P)
        G.ps = [T(stack.enter_context(nc.psum_tensor('ps%d' % i, [128, 512], F32)), 'ps%d' % i) for i in range(6)]
        G.pb = [T(stack.enter_context(nc.psum_tensor('pb%d' % i, [128, 1024], BF16)), 'pb%d' % i) for i in range(2)]
        if '0' in phases:
            phase0(G)
            P.barrier()
        for b in range(nseq):
            if 'A' in phases:
                phaseA(G, b)
                P.barrier()
            if 'C' in phases:
                phaseC(G, b)
                P.barrier()
            if 'D' in phases:
                phaseD(G, b)
                P.barrier()
            if 'E' in phases:
                phaseE(G, b)
                P.barrier()
        if 'F' in phases:
            phaseF(G)
            P.barrier()
        if not phases or 'F' not in phases:
            with ExitStack() as st:
                z = T(st.enter_context(nc.sbuf_tensor('zz', [128, D], F32)), 'zz')
                G.H.memset('dve', z[:], 0.0, [z])
                G.H.dma(G.out[0:128, :], z[:], [z], ['out'])
                P.barrier()
        P.emit()
    return nc


_UID = [0]


def sbt(G, st, name, shape, dt):
    _UID[0] += 1
    name = '%s_u%d' % (name, _UID[0])
    return T(st.enter_context(G.nc.sbuf_tensor(name, list(shape), dt)), name)


def phase0(G):
    H, I, Sx = G.H, G.I, G.S
    with ExitStack() as st:
        g = sbt(G, st, 'p0_g', [128, 8], F32)
        H.dma(g[:], I.g_mix.ap(), [], [g])
        stg = [sbt(G, st, 'p0_stg%d' % i, [128, NW], F32) for i in range(2)]
        wb = [sbt(G, st, 'p0_wb%d' % i, [128, NW], BF16) for i in range(2)]
        def load(kt):
            H.dma(stg[kt % 2][:], I.w_in[kt * 128:(kt + 1) * 128, :], [], [stg[kt % 2]])
        load(0)
        load(1)
        hw = NW // 2
        for kt in range(8):
            s_, w_ = stg[kt % 2], wb[kt % 2]
            H.ts('dve', w_[:, 0:hw], s_[:, 0:hw], g[:, kt:kt + 1], None, ALU.mult, None, [s_, g], [(w_, 0)])
            H.actf(w_[:, hw:NW], s_[:, hw:NW], AF.Copy, [s_, g], [(w_, 1)], scale=g[:, kt:kt + 1])
            if kt + 2 < 8:
                load(kt + 2)
            H.dma(Sx.Wbf[kt], w_[:], [(w_, 0), (w_, 1)], [('Wbf', kt)])
        zz = sbt(G, st, 'p0_zz', [128, 12 * D], BF16)
        H.memset('pool', zz[:], 0.0, [zz])
        for c8 in range(8):
            H.dma(Sx.xg[c8 * 1536:(c8 + 1) * 1536, :].rearrange("(p a) c -> p a c", a=12), zz[:].rearrange("p (a c) -> p a c", a=12),
                  [zz], [('xgz', c8)])
        jobs = [(I.w_br_mla, Sx.wbm, 0, 4, 'wbm'), (I.w_br_rwkv, Sx.wbr, 0, 4, 'wbr'), (I.w_out, Sx.wout, 0, 4, 'wout0'), (I.w_out, Sx.wout, 4, 4, 'wout1')]
        for ji, (src, dst, k0, nk, nm) in enumerate(jobs):
            s_, w_ = stg[ji % 2], wb[ji % 2]
            H.dma(s_[:, 0:nk * 1024].rearrange("p (k c) -> p k c", k=nk), src[k0 * 128:(k0 + nk) * 128, :].rearrange("(k p) c -> p k c", p=128),
                  [], [s_])
            H.cp('dve', w_[:, 0:2048], s_[:, 0:2048], [s_], [(w_, 0)])
            H.cp('act', w_[:, 2048:4096], s_[:, 2048:4096], [s_], [(w_, 1)])
            H.dma(dst.ap()[k0:k0 + nk].rearrange("k p c -> p k c"), w_[:, 0:nk * 1024].rearrange("p (k c) -> p k c", k=nk),
                  [(w_, 0), (w_, 1)], [(nm,)])


def phaseA(G, b):
    H, I, Sx = G.H, G.I, G.S
    with ExitStack() as st:
        ident = sbt(G, st, 'a_ident', [128, 128], BF16)
        H.dma(ident[:], I.ident.ap(), [], [ident])
        hT = sbt(G, st, 'a_hT', [128, 8, S], BF16)
        xt = [sbt(G, st, 'a_xt%d' % i, [128, D], F32) for i in range(2)]
        sq = sbt(G, st, 'a_sq', [128, D], F32)
        ss = [sbt(G, st, 'a_ss%d' % i, [128, 1], F32) for i in range(2)]
        xn = [sbt(G, st, 'a_xn%d' % i, [128, D], BF16) for i in range(2)]
        for i in range(16):
            x_, s_, n_ = xt[i % 2], ss[i % 2], xn[i % 2]
            pb = G.pb[i % 2]
            r0 = (b * 16 + i) * 128
            H.dma(x_[:], I.x[r0:r0 + 128, :], [], [x_], q='sp' if i % 2 == 0 else 'pool')
            H.memset('pool', s_[:], 0.0, [s_])
            P_ = G.P
            P_.act(lambda e, o=sq[:], a=x_[:], acc=s_[:]: e.activation(o, a, AF.Square, accum_out=acc), [x_, s_], [sq, s_])
            H.rsqrt(s_[:], s_[:], 1.0 / D, EPS, [s_], [s_])
            H.ts('dve', n_[:], x_[:], s_[:, 0:1], None, ALU.mult, None, [x_, s_], [n_])
            for kt in range(8):
                H.tr(pb[:, kt * 128:(kt + 1) * 128], n_[:, kt * 128:(kt + 1) * 128], ident[:], [n_, ident], [pb])
            H.cp('act', hT[:, :, i * 128:(i + 1) * 128], pb[:].rearrange("p (k t) -> p k t", k=8), [pb], [(hT, i)])
        for kt in range(8):
            H.dma(Sx.hT[b, kt], hT[:, kt, :], [(hT, i) for i in range(16)], [('hT_d', b, kt)],
                  q='sp' if kt % 2 == 0 else 'pool')


def phaseC(G, b):
    H, I, Sx = G.H, G.I, G.S
    ps = G.ps
    with ExitStack() as st:
        hT = sbt(G, st, 'c_hT', [128, 8, S], BF16)
        for kt in range(8):
            H.dma(hT[:, kt, :], Sx.hT[b, kt], [('hT_d', b, kt)], [(hT, kt)])
        hT_all = [(hT, kt) for kt in range(8)]
        Wm = sbt(G, st, 'c_Wm', [128, 8, 896], BF16)
        H.dma(Wm[:, :, 0:640], Sx.Wbf.ap()[:, :, 0:640].rearrange("k p c -> p k c"), [('Wbf', k) for k in range(8)], [(Wm, 0)])
        H.dma(Wm[:, :, 640:896], Sx.Wbf.ap()[:, :, KA_OFF:NW].rearrange("k p c -> p k c"), [('Wbf', k) for k in range(8)], [(Wm, 1)])
        Wm_all = [(Wm, 0), (Wm, 1)]
        gq = sbt(G, st, 'c_gq', [128, 3], F32)
        gkv = sbt(G, st, 'c_gkv', [128, 2], F32)
        H.dma(gq[:], I.g_q.ap(), [], [gq])
        H.dma(gkv[:], I.g_kv.ap(), [], [gkv])
        wuq = sbt(G, st, 'c_wuq', [128, 3, 2048], BF16)
        wukv = sbt(G, st, 'c_wukv', [128, 2, 2048], BF16)
        stg = sbt(G, st, 'c_stg', [128, 2048], F32)
        for j in range(3):
            H.dma(stg[:], I.wuq[j * 128:(j + 1) * 128, :], [], [stg])
            H.ts('dve', wuq[:, j, :], stg[:], gq[:, j:j + 1], None, ALU.mult, None, [stg, gq], [wuq])
        for j in range(2):
            H.dma(stg[:], I.wukv[j * 128:(j + 1) * 128, :], [], [stg])
            H.ts('dve', wukv[:, j, :], stg[:], gkv[:, j:j + 1], None, ALU.mult, None, [stg, gkv], [wukv])
        cosT = sbt(G, st, 'c_cos', [128, S], F32)
        sinT = sbt(G, st, 'c_sin', [128, S], F32)
        H.dma(cosT[:], I.cos128.ap(), [], [cosT])
        H.dma(sinT[:], I.sin128.ap(), [], [sinT])
        ones = sbt(G, st, 'c_ones', [128, 128], BF16)
        H.dma(ones[:], I.ones.ap(), [], [ones])
        onespad = sbt(G, st, 'c_onespad', [128, 2, 128], BF16)
        H.dma(onespad[:], I.onespad.ap().rearrange("h p c -> p h c"), [], [onespad])
        cq = sbt(G, st, 'c_cq', [128, 3, S], BF16)
        ckv = sbt(G, st, 'c_ckv', [128, 2, S], BF16)
        o1T = sbt(G, st, 'c_o1T', [128, 4, S], BF16)
        zsb = sbt(G, st, 'c_zsb', [128, 3, 512], F32)
        sqb = sbt(G, st, 'c_sqb', [128, 3, 512], BF16)
        rr = sbt(G, st, 'c_rr', [128, 512], F32)
        pi = 0
        for tb in range(4):
            tsl = slice(tb * 512, (tb + 1) * 512)
            for (c0, ntile, n, dst) in ((0, 3, 384, cq), (384, 2, 256, ckv)):
                for j in range(ntile):
                    p_ = ps[pi % 4]
                    pi += 1
                    for kt in range(8):
                        H.mm(p_[:], Wm[:, kt, c0 + j * 128:c0 + (j + 1) * 128], hT[:, kt, tsl], kt == 0, kt == 7,
                             [(Wm, 0), (hT, kt)], [p_])
                    H.cp('act', zsb[:, j, :], p_[:], [p_], [(zsb, j)])
                    H.actf(sqb[:, j, :], zsb[:, j, :], AF.Square, [(zsb, j)], [(sqb, j)])
                p2 = ps[4 + (pi % 2)]
                for j in range(ntile):
                    H.mm(p2[:], ones[:], sqb[:, j, :], j == 0, j == ntile - 1, [ones, (sqb, j)], [p2])
                H.rsqrt_act(rr[:], p2[:], 1.0 / n, EPS, [p2], [rr])
                for j in range(ntile):
                    H.tt('dve', dst[:, j, tsl], zsb[:, j, :], rr[:], ALU.mult,
                         [(zsb, j), rr], [(dst, tb)])
        cq_all = [(cq, tb) for tb in range(4)]
        ckv_all = [(ckv, tb) for tb in range(4)]
        QT = [sbt(G, st, 'c_QT%d' % i, [128, S], BF16) for i in range(2)]
        KT = [sbt(G, st, 'c_KT%d' % i, [128, S], BF16) for i in range(2)]
        Vp = [sbt(G, st, 'c_Vp%d' % i, [128, 16, 128], BF16) for i in range(2)]
        krot = sbt(G, st, 'c_krot', [128, S], BF16)
        t1 = [sbt(G, st, 'c_t1%d' % i, [128, 512], F32) for i in range(2)]
        t2 = [sbt(G, st, 'c_t2%d' % i, [128, 512], F32) for i in range(2)]
        PT = [sbt(G, st, 'c_PT%d' % i, [128, 512], BF16) for i in range(4)]
        rs = [sbt(G, st, 'c_rs%d' % i, [128, 512], F32) for i in range(2)]
        H.memset('pool', Vp[0][:, :, 64:128], 1.0, [(Vp[0], 'ones')])
        H.memset('pool', Vp[1][:, :, 0:64], 1.0, [(Vp[1], 'ones')])
        cnt = {'ti': 0, 'pt': 0}
        for tb in range(4):
            tsl = slice(tb * 512, (tb + 1) * 512)
            pA, pB = ps[0], ps[1]
            for kt in range(8):
                H.mm(pA[:], Wm[:, kt, 640:768], hT[:, kt, tsl], kt == 0, kt == 7, [(Wm, 1), (hT, kt)], [pA])
            for kt in range(8):
                H.mm(pB[:], Wm[:, kt, 768:896], hT[:, kt, tsl], kt == 0, kt == 7, [(Wm, 1), (hT, kt)], [pB])
            a_, b_ = t1[cnt['ti'] % 2], t2[cnt['ti'] % 2]
            cnt['ti'] += 1
            H.tt('dve', a_[:], pA[:], cosT[:, tsl], ALU.mult, [pA, cosT], [a_])
            H.tt('dve', b_[:], pB[:], sinT[:, tsl], ALU.mult, [pB, sinT], [b_])
            H.tt('dve', krot[:, tsl], a_[:], b_[:], ALU.add, [a_, b_], [(krot, tb)])
        krot_all = [(krot, tb) for tb in range(4)]

        def prep_head(h):
            Q_, K_, V_ = QT[h % 2], KT[h % 2], Vp[h % 2]
            for tb in range(4):
                tsl = slice(tb * 512, (tb + 1) * 512)
                pA, pB = ps[0], ps[1]
                for j in range(3):
                    H.mm(pA[:], wuq[:, j, h * 128:(h + 1) * 128], cq[:, j, tsl], j == 0, j == 2, [wuq, (cq, tb)], [pA])
                for j in range(3):
                    H.mm(pB[:], wuq[:, j, 1024 + h * 128:1024 + (h + 1) * 128], cq[:, j, tsl], j == 0, j == 2, [wuq, (cq, tb)], [pB])
                a_, b_ = t1[cnt['ti'] % 2], t2[cnt['ti'] % 2]
                cnt['ti'] += 1
                H.tt('dve', a_[:], pA[:], cosT[:, tsl], ALU.mult, [pA, cosT], [a_])
                H.tt('dve', b_[:], pB[:], sinT[:, tsl], ALU.mult, [pB, sinT], [b_])
                H.tt('dve', Q_[:, tsl], a_[:], b_[:], ALU.add, [a_, b_], [(Q_, tb)])
                pK = ps[4 + tb % 2]
                for j in range(2):
                    H.mm(pK[0:64, :], wukv[:, j, h * 128:h * 128 + 64], ckv[:, j, tsl], j == 0, j == 1, [wukv, (ckv, tb)], [pK])
                H.cp('dve', K_[0:64, tsl], pK[0:64, :], [pK], [(K_, tb)])
            H.cp('dve', K_[64:128, :], krot[64:128, :], krot_all, [(K_, 'r')])
            vsl = slice((h % 2) * 64, (h % 2) * 64 + 64)
            for g in range(4):
                p_ = ps[2 + (g % 2)]
                for t4 in range(4):
                    tt_ = g * 4 + t4
                    for j in range(2):
                        H.mm(p_[:, t4 * 128:(t4 + 1) * 128], ckv[:, j, tt_ * 128:(tt_ + 1) * 128],
                             wukv[:, j, 1024 + h * 128:1024 + (h + 1) * 128], j == 0, j == 1, [(ckv, tt_ // 4), wukv], [p_])
                H.cp('dve', V_[:, g * 4:(g + 1) * 4, vsl], p_[:].rearrange("p (a c) -> p a c", a=4)[:, :, vsl], [p_], [(V_, g)])

        def attn_head(h):
            Q_, K_, V_ = QT[h % 2], KT[h % 2], Vp[h % 2]
            half = slice((h % 2) * 64, (h % 2) * 64 + 64)
            other = slice((1 - h % 2) * 64, (1 - h % 2) * 64 + 64)
            Kr = [(K_, tb) for tb in range(4)] + [(K_, 'r')]
            sbanks = (ps[0], ps[1], ps[4], ps[5])
            LAG = 2
            items = [(qb, kt) for qb in range(4) for kt in range(16)]
            pts = {}
            for i in range(len(items) + LAG):
                if i < len(items):
                    qb, kt = items[i]
                    pS_ = sbanks[i % 4]
                    H.mm(pS_[:], K_[:, kt * 128:(kt + 1) * 128], Q_[:, qb * 512:(qb + 1) * 512], True, True, Kr + [(Q_, qb)], [pS_])
                    pt = PT[cnt['pt'] % 4]
                    cnt['pt'] += 1
                    pts[i] = pt
                    H.actf(pt[:], pS_[:], AF.Exp, [pS_], [pt], scale=ATT_SCALE)
                j = i - LAG
                if j >= 0:
                    qb, kt = items[j]
                    qsl = slice(qb * 512, (qb + 1) * 512)
                    pO = ps[2 + (qb % 2)]
                    pt = pts.pop(j)
                    H.mm(pO[:], V_[:, kt, :], pt[:], kt == 0, kt == 15, [(V_, kt // 4), (V_, 'ones'), pt], [pO])
                    if kt == 15:
                        r_ = rs[qb % 2]
                        H.recip(r_[half, :], pO[other, :], [pO], [r_])
                        H.tt('dve', o1T[half, h // 2, qsl], pO[half, :], r_[half, :], ALU.mult, [pO, r_], [(o1T, h // 2, qb)])

        prep_head(0)
        for h in range(8):
            if h + 1 < 8:
                prep_head(h + 1)
            attn_head(h)
        for pr in range(4):
            H.dma(Sx.o1[b, pr], o1T[:, pr, :], [(o1T, pr, qb) for qb in range(4)], [('o1_d', b, pr)],
                  q='sp' if pr % 2 == 0 else 'pool')


LNX_EPS = 64e-5


def phaseD(G, b):
    H, I, Sx, P = G.H, G.I, G.S, G.P
    ps, pbk = G.ps, G.pb
    nc = G.nc
    with ExitStack() as st:
        wdT = sbt(G, st, 'd_wdT', [128, S], BF16)
        adT = sbt(G, st, 'd_adT', [128, S], BF16)
        gdT = sbt(G, st, 'd_gdT', [128, S], BF16)
        cst = sbt(G, st, 'd_cst', [128, 64], F32)
        H.dma(cst[:], I.rw_cst.ap(), [], [cst])
        MU_P, MU_N, KK_, KA_, RK_, LG_, LB_, A0_ = 0, 15, 30, 34, 38, 42, 46, 50
        c0 = sbt(G, st, 'd_c0', [128, 24], F32)
        H.tt('dve', c0[:, 0:15], cst[:, MU_P:MU_P + 15], cst[:, MU_N:MU_N + 15], ALU.add, [cst], [c0])
        H.ts('dve', c0[:, 0:15], c0[:, 0:15], -1.0, 1.0, ALU.mult, ALU.add, [c0], [c0])
        H.ts('dve', c0[:, 16:20], cst[:, KA_:KA_ + 4], -1.0, 1.0, ALU.mult, ALU.add, [cst], [c0])
        H.ts('dve', c0[:, 20:24], cst[:, KA_:KA_ + 4], -2.0, 2.0, ALU.mult, ALU.add, [cst], [c0])
        bdones = sbt(G, st, 'd_bdones', [128, 128], BF16)
        H.dma(bdones[:], I.bdones.ap(), [], [bdones])
        ident = sbt(G, st, 'd_ident', [128, 128], BF16)
        H.dma(ident[:], I.ident.ap(), [], [ident])
        def mk0(name, shape, dt, n):
            return [[sbt(G, st, '%s_%d_%d' % (name, d, i), shape, dt) for i in range(n)] for d in range(2)]
        AR = mk0('d_AR', [128, 4, 2, 256], BF16, 2)
        Bt = mk0('d_Bt', [128, 4, 2, 128], BF16, 1)
        Kt = mk0('d_Kt', [128, 4, 2, 128], BF16, 1)
        Bh = mk0('d_Bh', [128, 4, 2, 128], BF16, 1)
        Kh = mk0('d_Kh', [128, 4, 2, 128], BF16, 1)
        Vb = mk0('d_Vb', [128, 4, 2, 128], BF16, 1)
        H32 = mk0('d_H32', [128, 4, 128], F32, 1)
        Hbf = mk0('d_Hbf', [128, 4, 128], BF16, 1)
        for d in range(2):
            for lst in (AR, Bt, Kt, Bh, Kh, Vb, H32, Hbf):
                for t_ in lst[d]:
                    H.memset('pool', t_[:], 0.0, [t_])
        with ExitStack() as s1:
            hT = sbt(G, s1, 'd_hT', [128, 8, S], BF16)
            for kt in range(8):
                H.dma(hT[:, kt, :], Sx.hT[b, kt], [('hT_d', b, kt)], [(hT, kt)])
            Wr = sbt(G, s1, 'd_Wr', [128, 8, 1920], BF16)
            H.dma(Wr[:, :, 0:960], Sx.Wbf.ap()[:, :, 672:1632].rearrange("k p c -> p k c"), [('Wbf', k) for k in range(8)], [(Wr, 0)])
            H.dma(Wr[:, :, 960:1920], Sx.Wbf.ap()[:, :, 1632:2592].rearrange("k p c -> p k c"), [('Wbf', k) for k in range(8)], [(Wr, 1)])
            zt = [sbt(G, s1, 'd_zt%d' % i, [128, S + 2], F32) for i in range(2)]
            acc = [sbt(G, s1, 'd_acc%d' % i, [128, S], F32) for i in range(2)]
            kf = sbt(G, s1, 'd_kf', [128, S], F32)
            sqk = sbt(G, s1, 'd_sqk', [128, S], BF16)
            rk = [sbt(G, s1, 'd_rk%d' % i, [128, 512], F32) for i in range(2)]
            ob = [sbt(G, s1, 'd_ob%d' % i, [128, S], BF16) for i in range(3)]
            obi = [0]

            def emit_out(qi, j, producer):
                o_ = ob[obi[0] % 3]
                obi[0] += 1
                producer(o_)
                H.dma(Sx.rw[b, qi, j], o_[:], [o_] + [(o_, t4) for t4 in range(4)], [('rw', b, qi, j)])
            for z_ in zt:
                H.memset('pool', z_[:, 0:1], 0.0, [(z_, 'l')])
                H.memset('pool', z_[:, S + 1:S + 2], 0.0, [(z_, 'r')])
            pi = 0
            for ct in range(15):
                z_, a_ = zt[ct % 2], acc[ct % 2]
                for tb in range(4):
                    p_ = ps[pi % 4]
                    pi += 1
                    for kt in range(8):
                        H.mm(p_[:], Wr[:, kt, ct * 128:(ct + 1) * 128], hT[:, kt, tb * 512:(tb + 1) * 512], kt == 0, kt == 7,
                             [(Wr, 0), (Wr, 1), (hT, kt)], [p_])
                    H.cp('act', z_[:, 1 + tb * 512:1 + (tb + 1) * 512], p_[:], [p_], [(z_, tb)])
                    H.actf(a_[:, tb * 512:(tb + 1) * 512], p_[:], AF.Copy, [p_, c0, a_], [a_], scale=c0[:, ct:ct + 1])
                zall = [(z_, tb) for tb in range(4)] + [(z_, 'l'), (z_, 'r')]
                H.stt('pool', a_[:], z_[:, 0:S], cst[:, MU_P + ct:MU_P + ct + 1], a_[:], ALU.mult, ALU.add, zall + [cst, a_], [a_])
                if ct < 4:
                    emit_out(0, ct, lambda o_: H.stt('dve', o_[:], z_[:, 2:S + 2], cst[:, MU_N + ct:MU_N + ct + 1], a_[:], ALU.mult, ALU.add, zall + [cst, a_], [o_]))
                elif ct < 8:
                    j = ct - 4
                    H.stt('dve', kf[:], z_[:, 2:S + 2], cst[:, MU_N + ct:MU_N + ct + 1], a_[:], ALU.mult, ALU.add, zall + [cst, a_], [kf])
                    emit_out(3, j, lambda o_: H.cp('act', o_[:], kf[:], [kf], [o_]))
                    H.ts('dve', kf[:], kf[:], cst[:, KK_ + j:KK_ + j + 1], None, ALU.mult, None, [kf, cst], [kf])
                    def kkprod(o_):
                        H.actf(sqk[:], kf[:], AF.Square, [kf], [sqk])
                        for tb in range(4):
                            tsl = slice(tb * 512, (tb + 1) * 512)
                            p2 = ps[4 + tb % 2]
                            rk_ = rk[tb % 2]
                            H.mm(p2[:], bdones[:], sqk[:, tsl], True, True, [bdones, sqk], [p2])
                            H.rsqrt_act(rk_[:], p2[:], 1.0, 1e-24, [p2], [rk_])
                            H.tt('dve', o_[:, tsl], kf[:, tsl], rk_[:], ALU.mult, [kf, rk_], [(o_, tb)])
                    emit_out(2, j, kkprod)
                elif ct < 12:
                    emit_out(1, ct - 8, lambda o_: H.stt('dve', o_[:], z_[:, 2:S + 2], cst[:, MU_N + ct:MU_N + ct + 1], a_[:], ALU.mult, ALU.add, zall + [cst, a_], [o_]))
                else:
                    H.stt('dve', a_[:], z_[:, 2:S + 2], cst[:, MU_N + ct:MU_N + ct + 1], a_[:], ALU.mult, ALU.add, zall + [cst, a_], [a_])
                    if ct == 12:
                        H.actf(wdT[:], a_[:], AF.Tanh, [a_], [wdT])
                    elif ct == 13:
                        H.cp('act', adT[:], a_[:], [a_], [adT])
                    else:
                        H.actf(gdT[:], a_[:], AF.Sigmoid, [a_], [gdT])
        P.barrier()
        yT = sbt(G, st, 'd_yT', [128, 4, S], F32)
        H.memset('pool', yT[:], 0.0, [yT])
        with ExitStack() as s2:
            stg = sbt(G, s2, 'd_stg', [128, 512], F32)
            wup = sbt(G, s2, 'd_wup', [128, 512], BF16)
            aup = sbt(G, s2, 'd_aup', [128, 512], BF16)
            H.dma(stg[:], I.w_up.ap(), [], [stg])
            H.cp('dve', wup[:], stg[:], [stg], [wup])
            H.dma(stg[:], I.a_up.ap(), [], [stg])
            H.cp('dve', aup[:], stg[:], [stg], [aup])
            w0b = sbt(G, s2, 'd_w0b', [128, 2, 512], F32)
            H.dma(w0b[:], I.w0b.ap().rearrange("d p c -> p d c"), [], [w0b])
            tri3 = sbt(G, s2, 'd_tri3', [128, 2, 384], F32)
            H.dma(tri3[:], I.tri3.ap().rearrange("d p c -> p d c"), [], [tri3])
            maskT = sbt(G, s2, 'd_maskT', [128, 2, 512], BF16)
            H.dma(maskT[:], I.maskT.ap().rearrange("d p c -> p d c"), [], [maskT])
            maskL = sbt(G, s2, 'd_maskL', [128, 2, 512], BF16)
            H.dma(maskL[:], I.maskL.ap().rearrange("d p c -> p d c"), [], [maskL])
            ident4 = sbt(G, s2, 'd_ident4', [128, 512], BF16)
            H.dma(ident4[:], I.ident4.ap(), [], [ident4])

            def mk(name, shape, dt, n):
                return [[sbt(G, s2, '%s_%d_%d' % (name, d, i), shape, dt) for i in range(n)] for d in range(2)]
            sg = mk('d_sg', [128, 512], F32, 1)
            E3 = mk('d_E3', [128, 4, 384], F32, 1)
            ld = mk('d_ld', [128, 4, 4, 128], BF16, 1)
            fac = mk('d_fac', [128, 4, 128], F32, 1)
            Einv = mk('d_Einv', [128, 4, 128], F32, 1)
            a_t = mk('d_at', [128, 4, 128], F32, 1)
            bq = mk('d_bq', [128, 4, 128], BF16, 1)
            kd = mk('d_kd', [128, 4, 128], BF16, 1)
            gC = mk('d_gC', [128, 4, 2], F32, 2)
            TTb = mk('d_TTb', [128, 4, 128], BF16, 2)
            E1 = mk('d_E1', [128, 4, 256], BF16, 2)
            E2 = mk('d_E2', [128, 4, 256], BF16, 2)
            E3l = mk('d_E3l', [128, 4, 128], BF16, 2)
            QL = mk('d_QL', [128, 4, 256], BF16, 2)
            Ak = mk('d_Ak', [128, 4, 128], BF16, 2)
            TM = mk('d_TM', [128, 4, 384], BF16, 2)
            Xs = mk('d_Xs', [128, 4, 128], BF16, 1)
            Us = mk('d_Us', [128, 4, 128], BF16, 1)
            Ys = mk('d_Ys', [128, 4, 128], F32, 1)
            ytmp = mk('d_ytmp', [128, 4, 64], F32, 1)
            unit = [0]

            def nextbank():
                u = unit[0]
                unit[0] += 1
                return (ps[0], ps[1], ps[4], ps[5])[u % 4]
            ecnt = [0]

            def eng2():
                ecnt[0] += 1
                return 'dve'

            def prep_tile(d, tt):
                par = tt % 2
                tsl = slice(tt * 128, (tt + 1) * 128)
                dsl = slice(d * 64, (d + 1) * 64)
                sg_, tw_, E3_, Ei_, at_, bq_, kd_ = sg[d][0], sg[d][0], E3[d][0], Einv[d][0], a_t[d][0], bq[d][0], kd[d][0]
                ld_ = ld[d][0]
                for qi in range(4):
                    H.dma(ld_[:, qi, :, :], Sx.rw.ap()[b, qi, :, :, tsl].rearrange("h p t -> p h t"),
                          [('rw', b, qi, j) for j in range(4)], [(ld_, qi)])
                r_t, v_t, kk_t, k_t = ld_[:, 0], ld_[:, 1], ld_[:, 2], ld_[:, 3]
                fac_ = fac[d][0]
                pw = nextbank()
                H.mm(pw[:], wdT[dsl, tsl], wup[dsl, :], True, True, [wdT, wup], [pw])
                H.tt('dve', tw_[:], pw[:], w0b[:, d, :], ALU.add, [pw, w0b], [tw_])
                H.actf(sg_[:], tw_[:], AF.Sigmoid, [tw_], [sg_])
                for hp in range(4):
                    pc = nextbank()
                    H.mm(pc[:, 0:384], sg_[:, hp * 128:(hp + 1) * 128], tri3[:, d, :], True, True, [sg_, tri3], [pc])
                    H.actf(E3_[:, hp, :], pc[:, 0:384], AF.Exp, [pc], [(E3_, hp)])
                    H.actf(Ei_[:, hp, :], pc[:, 0:128], AF.Exp, [pc], [(Ei_, hp)], scale=-1.0)
                E3all = [(E3_, hp) for hp in range(4)]
                Eiall = [(Ei_, hp) for hp in range(4)]
                pa = nextbank()
                for hp in range(4):
                    H.mm(pa[:, hp * 128:(hp + 1) * 128], aup[dsl, hp * 128:(hp + 1) * 128], adT[dsl, tsl], True, True, [aup, adT], [pa])
                for hp in range(4):
                    H.actf(at_[:, hp, :], pa[:, hp * 128:(hp + 1) * 128], AF.Sigmoid, [pa, cst], [(at_, hp)],
                           bias=cst[:, A0_ + d * 4 + hp:A0_ + d * 4 + hp + 1])
                atall = [(at_, hp) for hp in range(4)]
                if getattr(P, '_cap', None) is not None:
                    P._cap.append('MARK')
                H.tt('pool', bq_[:], kk_t, at_[:], ALU.mult, [(ld_, 2)] + atall, [bq_])
                for hp in range(4):
                    H.ts(eng2(), fac_[:, hp, :], at_[:, hp, :], cst[:, KA_ + hp:KA_ + hp + 1], c0[:, 16 + hp:17 + hp], ALU.mult, ALU.add,
                         [(at_, hp), cst, c0], [(fac_, hp)])
                H.tt('dve', kd_[:], k_t, fac_[:], ALU.mult, [(ld_, 3)] + [(fac_, hp) for hp in range(4)], [kd_])
                AR_, Bt_, Kt_, Bh_, Kh_, Vb_ = AR[d][par], Bt[d][0], Kt[d][0], Bh[d][0], Kh[d][0], Vb[d][0]
                gcols = (63, 127) if d == 0 else (0, 64)
                for c2_ in range(2):
                    H.cp('pool', gC[d][par][:, :, c2_:c2_ + 1], E3_[:, :, gcols[c2_]:gcols[c2_] + 1], E3all, [gC[d][par]])

                def v4(ap):
                    return ap.rearrange("p h (c t) -> p h c t", c=2)
                for hf in range(2):
                    psl = slice(hf * 64, hf * 64 + 64)
                    csl = slice(hf * 64, hf * 64 + 64)
                    gi, gp, gs = E3_[psl, :, 0:128], E3_[psl, :, 128:256], E3_[psl, :, 256:384]
                    H.stt(eng2(), AR_[psl, :, :, csl], v4(ld_[psl, 2]), -1.0, v4(gp), ALU.mult, ALU.mult,
                          [(ld_, 2)] + E3all, [(AR_, hf, 0)])
                    H.tt(eng2(), AR_[psl, :, :, 128 + hf * 64:128 + hf * 64 + 64], v4(ld_[psl, 0]), v4(gi), ALU.mult,
                         [(ld_, 0)] + E3all, [(AR_, hf, 1)])
                    H.tt(eng2(), Bt_[psl, :, :, csl], v4(bq_[psl, :, :]), v4(Ei_[psl, :, :]), ALU.mult, [bq_] + Eiall, [(Bt_, hf)])
                    H.tt(eng2(), Kt_[psl, :, :, csl], v4(kd_[psl, :, :]), v4(Ei_[psl, :, :]), ALU.mult, [kd_] + Eiall, [(Kt_, hf)])
                    H.cp(eng2(), Vb_[psl, :, :, csl], v4(ld_[psl, 1]), [(ld_, 1)], [(Vb_, hf)])

            def res2(t_):
                return [(t_, 0), (t_, 1)]

            def rARf(AR_):
                return [(AR_, 0, 0), (AR_, 1, 0), (AR_, 0, 1), (AR_, 1, 1)]

            def P0(d, tt, c2, n):
                par, np_ = tt % 2, n % 2
                AR_, Bt_, Kt_ = AR[d][par], Bt[d][0], Kt[d][0]
                rAR = rARf(AR_)
                for (lt, dst) in ((Bt_, E1[d][np_]), (Kt_, E2[d][np_])):
                    for pr in range(2):
                        pk = nextbank()
                        for q in range(2):
                            hp = pr * 2 + q
                            H.mm(pk[:, q * 256:(q + 1) * 256], lt[:, hp, c2, :], AR_[:, hp, c2, :], True, True, res2(lt) + rAR, [pk])
                        H.tt('dve', dst[:, pr * 2:pr * 2 + 2, :], pk[:].rearrange("p (a c) -> p a c", a=2),
                             maskT[:, d, :].rearrange("p (a c) -> p a c", a=2), ALU.mult, [pk, maskT], [(dst, pr)])
                pk = nextbank()
                for hp in range(4):
                    H.mm(pk[:, hp * 128:(hp + 1) * 128], AR_[:, hp, c2, 0:128], Bt_[:, hp, c2, :], True, True, res2(Bt_) + rAR, [pk])
                H.tt('dve', E3l[d][np_][:], pk[:].rearrange("p (a c) -> p a c", a=4),
                     maskL[:, d, :].rearrange("p (a c) -> p a c", a=4), ALU.mult, [pk, maskL], [E3l[d][np_]])
                H.tt('pool', Ak[d][0][:], E1[d][np_][:, :, 0:128], ident4[:].rearrange("p (a c) -> p a c", a=4), ALU.add,
                     [(E1[d][np_], 0), (E1[d][np_], 1), ident4], [Ak[d][0]])
                Bh_, Kh_, Vb_ = Bt[d][0], Kt[d][0], Vb[d][0]
                tm = TM[d][np_]
                pb0, pb1 = pbk[0], pbk[1]
                for hp in range(4):
                    H.tr(pb0[:, hp * 256:hp * 256 + 128], Bh_[:, hp, c2, :], ident[:], res2(Bh_) + [ident], [pb0])
                    H.tr(pb0[:, hp * 256 + 128:hp * 256 + 256], Kh_[:, hp, c2, :], ident[:], res2(Kh_) + [ident], [pb0])
                H.cp('act', tm[:, :, 0:256], pb0[:].rearrange("p (a c) -> p a c", a=4), [pb0], [(tm, 0)])
                for hp in range(4):
                    H.tr(pb1[:, hp * 128:(hp + 1) * 128], Vb_[:, hp, c2, :], ident[:], res2(Vb_) + [ident], [pb1])
                H.cp('act', tm[:, :, 256:384], pb1[:, 0:512].rearrange("p (a c) -> p a c", a=4), [pb1], [(tm, 1)])

            def PQ(d, n, k):
                np_ = n % 2
                e1, e3l = E1[d][np_], E3l[d][np_]
                qprev = QL[d][(k - 1) % 2]
                qcur = QL[d][k % 2]

                def Qp(hp):
                    return e1[:, hp, 0:128] if k == 1 else qprev[:, hp, 0:128]

                def Lp(hp):
                    return e3l[:, hp, :] if k == 1 else qprev[:, hp, 128:256]
                for pr in range(2):
                    rprev = [(e1, pr), e3l] if k == 1 else [(qprev, pr)]
                    pk = nextbank()
                    for q in range(2):
                        hp = pr * 2 + q
                        if k < 5:
                            H.mm(pk[:, q * 256:q * 256 + 128], Lp(hp), Qp(hp), True, True, rprev, [pk])
                        H.mm(pk[:, q * 256 + 128:q * 256 + 256], Qp(hp), Lp(hp), True, True, rprev, [pk])
                    if k < 5:
                        H.cp('act', qcur[:, pr * 2:pr * 2 + 2, :], pk[:].rearrange("p (a c) -> p a c", a=2), [pk], [(qcur, pr)])
                    else:
                        H.cp('act', qcur[:, pr * 2:pr * 2 + 2, 128:256], pk[:].rearrange("p (a c) -> p a c", a=2)[:, :, 128:256], [pk], [(qcur, pr)])

            def PA(d, n, k):
                qcur = QL[d][k % 2]
                aprev = Ak[d][(k - 1) % 2]
                acur = Ak[d][k % 2] if k < 5 else TTb[d][n % 2]
                pk = nextbank()
                for hp in range(4):
                    H.mm(pk[:, hp * 128:(hp + 1) * 128], qcur[:, hp, 128:256], aprev[:, hp, :], True, True, [(qcur, 0), (qcur, 1), aprev], [pk])
                H.tt('dve', acur[:], pk[:].rearrange("p (a c) -> p a c", a=4), aprev[:], ALU.add, [pk, aprev], [acur])

            def S0(d, tt, c2, n):
                par, np_ = tt % 2, n % 2
                AR_ = AR[d][par]
                e2, tm = E2[d][np_], TM[d][np_]
                SX = ps[2 + d]
                hbf, xs = Hbf[d][0], Xs[d][0]
                for hp in range(4):
                    o = SX[:, hp * 128:(hp + 1) * 128]
                    H.mm(o, AR_[:, hp, c2, 0:128], hbf[:, hp, :], True, False, rARf(AR_) + [hbf], [SX])
                    H.mm(o, e2[:, hp, 0:128], tm[:, hp, 256:384], False, True, [(e2, 0), (e2, 1), (tm, 1)], [SX])
                H.cp('act', xs[:], SX[:].rearrange("p (a c) -> p a c", a=4), [SX], [xs])

            def S1(d, tt, c2, n):
                SX = ps[2 + d]
                TT_ = TTb[d][n % 2]
                xs, us = Xs[d][0], Us[d][0]
                for hp in range(4):
                    H.mm(SX[:, hp * 128:(hp + 1) * 128], TT_[:, hp, :], xs[:, hp, :], True, True, [TT_, xs], [SX])
                H.cp('act', us[:], SX[:].rearrange("p (a c) -> p a c", a=4), [SX], [us])

            def S2(d, tt, c2, n):
                par, np_ = tt % 2, n % 2
                AR_ = AR[d][par]
                rAR = rARf(AR_)
                e1, e2, tm = E1[d][np_], E2[d][np_], TM[d][np_]
                SX, SH = ps[2 + d], ps[2 + d]
                hbf, h32, us, ys, yt_ = Hbf[d][0], H32[d][0], Us[d][0], Ys[d][0], ytmp[d][0]
                for hp in range(4):
                    o = SX[:, hp * 128:(hp + 1) * 128]
                    H.mm(o, hbf[:, hp, :], AR_[:, hp, c2, 128:256], True, False, rAR + [hbf], [SX])
                    H.mm(o, us[:, hp, :], e1[:, hp, 128:256], False, False, [us, (e1, 0), (e1, 1)], [SX])
                    H.mm(o, tm[:, hp, 256:384], e2[:, hp, 128:256], False, True, [(tm, 1), (e2, 0), (e2, 1)], [SX])
                H.cp('act', ys[:], SX[:].rearrange("p (a c) -> p a c", a=4), [SX], [ys])
                for hp in range(4):
                    o = SH[:, hp * 128:(hp + 1) * 128]
                    H.mm(o, tm[:, hp, 0:128], us[:, hp, :], True, False, [(tm, 0), us], [SH])
                    H.mm(o, tm[:, hp, 128:256], tm[:, hp, 256:384], False, True, [(tm, 0), (tm, 1)], [SH])
                csl = slice(tt * 128 + c2 * 64, tt * 128 + c2 * 64 + 64)
                H.tt('pool', yt_[:], ys[:, :, 0:64], ys[:, :, 64:128], ALU.add, [ys], [yt_])
                H.tt('pool', yT[:, :, csl], yT[:, :, csl], yt_[:], ALU.add, [yT, yt_], [yT])
                gC_ = gC[d][par]
                H.tt('dve', h32[:], h32[:], SH[:].rearrange("p (a c) -> p a c", a=4), ALU.add, [h32, SH], [h32])
                for hp in range(4):
                    H.ts('dve', h32[:, hp, :], h32[:, hp, :], gC_[:, hp, c2:c2 + 1], None, ALU.mult, None, [h32, gC_], [h32])
                H.cp('pool', hbf[:], h32[:], [h32], [hbf])

            def step_info(n):
                i, c = n // 2, n % 2
                return [(i, c), (15 - i, 1 - c)]

            def capture(fn):
                ops = []
                P._cap = ops
                P.add = lambda E, f, r=(), w=(), dma=False: ops.append((E, f, r, w, dma))
                try:
                    fn()
                finally:
                    del P.add
                    P._cap = None
                return ops

            pending = []

            def drip(frac_left):
                if not pending:
                    return
                k = -(-len(pending) // max(frac_left, 1))
                for _ in range(min(k, len(pending))):
                    P.add(*pending.pop(0))

            for d in range(2):
                prep_tile(d, (0, 15)[d])
            NS = 32 if 'noD2' not in G.dbg else 0
            for n in range(-1, NS):
                nx = n + 1
                if nx < NS:
                    inf = step_info(nx)
                    for d in range(2):
                        P0(d, inf[d][0], inf[d][1], nx)
                if n >= 0:
                    cur = step_info(n)
                    for d in range(2):
                        S0(d, cur[d][0], cur[d][1], n)
                if n >= 0 and n % 2 == 0 and n + 2 < NS:
                    i2 = (n + 2) // 2
                    a_ops = capture(lambda: prep_tile(0, i2))
                    b_ops = capture(lambda: prep_tile(1, 15 - i2))
                    for lst in (a_ops, b_ops):
                        mk_ = lst.index('MARK')
                        for op_ in lst[:mk_]:
                            P.add(*op_)
                        del lst[:mk_ + 1]
                    m_ = max(len(a_ops), len(b_ops))
                    for j in range(m_):
                        if j < len(a_ops):
                            pending.append(a_ops[j])
                        if j < len(b_ops):
                            pending.append(b_ops[j])
                drip(6)
                if nx < NS:
                    for d in range(2):
                        PQ(d, nx, 1)
                if n >= 0:
                    for d in range(2):
                        S1(d, cur[d][0], cur[d][1], n)
                drip(5)
                if nx < NS:
                    for d in range(2):
                        PQ(d, nx, 2)
                    for d in range(2):
                        PA(d, nx, 1)
                if n >= 0:
                    for d in range(2):
                        S2(d, cur[d][0], cur[d][1], n)
                drip(4)
                if nx < NS:
                    for k in (3, 4, 5):
                        for d in range(2):
                            PQ(d, nx, k)
                        for d in range(2):
                            PA(d, nx, k - 1)
                        drip(6 - k)
                    for d in range(2):
                        PA(d, nx, 5)
                while pending:
                    P.add(*pending.pop(0))
        P.barrier()
        with ExitStack() as s3:
            bdf = sbt(G, s3, 'd_bdf', [128, 128], F32)
            H.dma(bdf[:], I.bdones_f.ap(), [], [bdf])
            stg = sbt(G, s3, 'd_stg3', [128, 512], F32)
            gup = sbt(G, s3, 'd_gup', [128, 512], BF16)
            H.dma(stg[:], I.g_up.ap(), [], [stg])
            H.cp('dve', gup[:], stg[:], [stg], [gup])
            o2T = sbt(G, s3, 'd_o2T', [128, 4, S], BF16)
            NB3 = 3

            def m3(name, dt):
                return [sbt(G, s3, 'd_%s%d' % (name, i), [128, 512], dt) for i in range(NB3)]
            yc, sq, rs, bo, as0, as1 = m3('yc', F32), m3('sq3', F32), m3('rs3', F32), m3('bo', F32), m3('as0', F32), m3('as1', F32)
            sb_, kl, kds, rl, vl = m3('sb', BF16), m3('kl', BF16), m3('kds', BF16), m3('rl', BF16), m3('vl', BF16)
            aup3 = sbt(G, s3, 'd_aup3', [128, 512], BF16)
            H.dma(stg[:], I.a_up.ap(), [gup], [stg])
            H.cp('dve', aup3[:], stg[:], [stg], [aup3])

            def d3_block(bi):
                hp, tb = bi // 4, bi % 4
                c = bi % NB3
                tsl = slice(tb * 512, (tb + 1) * 512)
                rl_, vl_, kl_ = rl[c], vl[c], kl[c]
                y_, q_, r_, s_, b_, a0_, a1_, kd_ = yc[c], sq[c], rs[c], sb_[c], bo[c], as0[c], as1[c], kds[c]
                bA, bB = ps[2 * c], ps[2 * c + 1]
                H.dma(rl_[:], Sx.rw[b, 0, hp, :, tsl], [('rw', b, 0, hp)], [rl_])
                H.dma(vl_[:], Sx.rw[b, 1, hp, :, tsl], [('rw', b, 1, hp)], [vl_])
                H.dma(kl_[:], Sx.rw[b, 3, hp, :, tsl], [('rw', b, 3, hp)], [kl_])
                H.mm(bA[:], bdf[:], yT[:, hp, tsl], True, True, [bdf, yT], [bA])
                H.mm(bB[:], aup3[0:64, hp * 128:(hp + 1) * 128], adT[0:64, tsl], True, True, [aup3, adT], [bB])
                H.stt('dve', y_[:], bA[:], -1.0 / 64, yT[:, hp, tsl], ALU.mult, ALU.add, [bA, yT], [y_])
                H.actf(a0_[:], bB[:], AF.Sigmoid, [bB, cst], [a0_], bias=cst[:, A0_ + hp:A0_ + hp + 1])
                H.actf(q_[:], y_[:], AF.Square, [y_], [q_])
                H.mm(bB[:], aup3[64:128, hp * 128:(hp + 1) * 128], adT[64:128, tsl], True, True, [aup3, adT], [bB])
                H.mm(bA[:], bdf[:], q_[:], True, True, [bdf, q_], [bA])
                H.actf(a1_[:], bB[:], AF.Sigmoid, [bB, cst], [a1_], bias=cst[:, A0_ + 4 + hp:A0_ + 4 + hp + 1])
                H.rsqrt_act(r_[:], bA[:], 1.0 / 64, LNX_EPS, [bA], [r_])
                H.tt('pool', a0_[:], a0_[:], a1_[:], ALU.add, [a0_, a1_], [a0_])
                H.tt('pool', y_[:], y_[:], r_[:], ALU.mult, [y_, r_], [y_])
                H.ts('dve', a0_[:], a0_[:], cst[:, KA_ + hp:KA_ + hp + 1], c0[:, 20 + hp:21 + hp], ALU.mult, ALU.add, [a0_, cst, c0], [a0_])
                H.ts('dve', y_[:], y_[:], cst[:, LG_ + hp:LG_ + hp + 1], cst[:, LB_ + hp:LB_ + hp + 1], ALU.mult, ALU.add, [y_, cst], [y_])
                H.tt('pool', kd_[:], kl_[:], a0_[:], ALU.mult, [kl_, a0_], [kd_])
                H.stt('dve', s_[:], rl_[:], cst[:, RK_ + hp:RK_ + hp + 1], kd_[:], ALU.mult, ALU.mult, [rl_, cst, kd_], [s_])
                H.mm(bB[:], bdones[:], s_[:], True, True, [bdones, s_], [bB])
                H.mm(bA[:], gup[:, hp * 128:(hp + 1) * 128], gdT[:, tsl], True, True, [gup, gdT], [bA])
                H.tt('dve', b_[:], bB[:], vl_[:], ALU.mult, [bB, vl_], [b_])
                H.tt('pool', y_[:], y_[:], b_[:], ALU.add, [y_, b_], [y_])
                H.tt('dve', o2T[:, hp, tsl], bA[:], y_[:], ALU.mult, [bA, y_], [(o2T, hp)])

            def capture3(fn):
                ops = []
                P.add = lambda E, f, r=(), w=(), dma=False: ops.append((E, f, r, w, dma))
                try:
                    fn()
                finally:
                    del P.add
                return ops

            NBLK = 16 if 'noD3' not in G.dbg else 0
            for g0 in range(0, NBLK, NB3):
                chains = [capture3(lambda bi=bi: d3_block(bi)) for bi in range(g0, min(g0 + NB3, NBLK))]
                idx = [0] * len(chains)
                live = True
                while live:
                    live = False
                    for ci, ops_ in enumerate(chains):
                        if idx[ci] < len(ops_):
                            P.add(*ops_[idx[ci]])
                            idx[ci] += 1
                            live = True
            for pr in range(4):
                H.dma(Sx.o2[b, pr], o2T[:, pr, :], [(o2T, pr)], [('o2_d', b, pr)], q='sp' if pr % 2 == 0 else 'pool')


CAP = 384
NSLOT = CAP // 128


def phaseE(G, b):
    H, I, Sx, P = G.H, G.I, G.S, G.P
    ps = G.ps
    with ExitStack() as st:
        hT = sbt(G, st, 'e_hT', [128, 8, S], BF16)
        for kt in range(8):
            H.dma(hT[:, kt, :], Sx.hT[b, kt], [('hT_d', b, kt)], [(hT, kt)])
        o1T = sbt(G, st, 'e_o1T', [128, 4, S], BF16)
        o2T = sbt(G, st, 'e_o2T', [128, 4, S], BF16)
        for pr in range(4):
            H.dma(o1T[:, pr, :], Sx.o1[b, pr], [('o1_d', b, pr)], [(o1T, pr)])
            H.dma(o2T[:, pr, :], Sx.o2[b, pr], [('o2_d', b, pr)], [(o2T, pr)])
        Wg = sbt(G, st, 'e_Wg', [128, 8, 2048], BF16)
        H.dma(Wg[:, :, 0:1024], Sx.Wbf.ap()[:, :, 2592:3616].rearrange("k p c -> p k c"), [('Wbf', k) for k in range(8)], [(Wg, 0)])
        H.dma(Wg[:, :, 1024:2048], Sx.Wbf.ap()[:, :, 3616:4640].rearrange("k p c -> p k c"), [('Wbf', k) for k in range(8)], [(Wg, 1)])
        wbm = sbt(G, st, 'e_wbm', [128, 4, 1024], BF16)
        wbr = sbt(G, st, 'e_wbr', [128, 4, 1024], BF16)
        wout = sbt(G, st, 'e_wout', [128, 8, 1024], BF16)
        H.dma(wbm[:], Sx.wbm.ap().rearrange("k p c -> p k c"), [('wbm',)], [wbm])
        H.dma(wbr[:], Sx.wbr.ap().rearrange("k p c -> p k c"), [('wbr',)], [wbr])
        H.dma(wout[:], Sx.wout.ap().rearrange("k p c -> p k c"), [('wout0',), ('wout1',)], [wout])
        gb = sbt(G, st, 'e_gb', [128, 16], F32)
        H.dma(gb[:], I.gate_b.ap(), [], [gb])
        mT = [sbt(G, st, 'e_mT%d' % i, [128, 8, 512], BF16) for i in range(2)]
        g1 = [sbt(G, st, 'e_g1%d' % i, [128, 512], F32) for i in range(2)]
        g2 = [sbt(G, st, 'e_g2%d' % i, [128, 512], F32) for i in range(2)]
        xt = [sbt(G, st, 'e_xt%d' % i, [128, D], F32) for i in range(2)]
        it = 0
        xi = 0
        for tb in range(4):
            tsl = slice(tb * 512, (tb + 1) * 512)
            m_ = mT[tb % 2]
            for dt in range(8):
                dsl = slice(dt * 128, (dt + 1) * 128)
                pg1, pg2, pb1, pb2 = ps[0], ps[1], ps[2], ps[3]
                for kt in range(8):
                    H.mm(pg1[:], Wg[:, kt, dsl], hT[:, kt, tsl], kt == 0, kt == 7, [(Wg, 0), (hT, kt)], [pg1])
                for kt in range(8):
                    H.mm(pg2[:], Wg[:, kt, 1024 + dt * 128:1024 + (dt + 1) * 128], hT[:, kt, tsl], kt == 0, kt == 7, [(Wg, 1), (hT, kt)], [pg2])
                for j in range(4):
                    H.mm(pb1[:], wbm[:, j, dsl], o1T[:, j, tsl], j == 0, j == 3, [wbm, (o1T, j)], [pb1])
                for j in range(4):
                    H.mm(pb2[:], wbr[:, j, dsl], o2T[:, j, tsl], j == 0, j == 3, [wbr, (o2T, j)], [pb2])
                a_, b_ = g1[it % 2], g2[it % 2]
                it += 1
                H.actf(a_[:], pg1[:], AF.Sigmoid, [pg1, gb], [a_], bias=gb[:, dt:dt + 1])
                H.actf(b_[:], pg2[:], AF.Sigmoid, [pg2, gb], [b_], bias=gb[:, 8 + dt:9 + dt])
                H.tt('dve', a_[:], a_[:], pb1[:], ALU.mult, [a_, pb1], [a_])
                H.tt('dve', b_[:], b_[:], pb2[:], ALU.mult, [b_, pb2], [b_])
                H.tt('dve', m_[:, dt, :], a_[:], b_[:], ALU.add, [a_, b_], [(m_, dt)])
            for t4 in range(4):
                x_ = xt[xi % 2]
                xi += 1
                r0 = b * S + tb * 512 + t4 * 128
                H.dma(x_[:], I.x[r0:r0 + 128, :], [], [x_])
                for half in range(2):
                    po = ps[4 + half]
                    for dt in range(8):
                        H.mm(po[:], m_[:, dt, t4 * 128:(t4 + 1) * 128], wout[:, dt, half * 512:(half + 1) * 512], dt == 0, dt == 7,
                             [(m_, dt), wout], [po])
                    H.tt('dve', x_[:, half * 512:(half + 1) * 512], x_[:, half * 512:(half + 1) * 512], po[:], ALU.add, [x_, po], [x_])
                H.dma(Sx.x1[r0:r0 + 128, :], x_[:], [x_], [('x1', r0)], q='pool')


def phaseF(G):
    H, I, Sx, P = G.H, G.I, G.S, G.P
    ps, pbk = G.ps, G.pb
    nc = G.nc
    NT = G.nseq * 16
    IOA = bass.IndirectOffsetOnAxis
    with ExitStack() as st:
        slots = [sbt(G, st, 'f_slot%d' % k, [128, NT], I32) for k in range(2)]
        cw = [sbt(G, st, 'f_cw%d' % k, [128, NT], F32) for k in range(2)]
        gfb = sbt(G, st, 'f_gfb', [128, 8, 256], F32)
        H.dma(gfb[:], I.gffn_b.ap(), [], [gfb])
        ident = sbt(G, st, 'f_ident', [128, 128], BF16)
        H.dma(ident[:], I.ident.ap(), [], [ident])
        with ExitStack() as s1:
            identf = sbt(G, s1, 'f_identf', [128, 128], F32)
            H.dma(identf[:], I.ident_f.ap(), [], [identf])
            ones = sbt(G, s1, 'f_ones', [128, 128], BF16)
            H.dma(ones[:], I.ones.ap(), [], [ones])
            tris = sbt(G, s1, 'f_tris', [128, 128], BF16)
            H.dma(tris[:], I.tris.ap(), [], [tris])
            RW = sbt(G, s1, 'f_RW', [128, 8, 36], F32)
            H.dma(RW[:], I.rw_router.ap(), [], [RW])
            H.tt('dve', RW[:], RW[:], gfb[:, :, 0:36], ALU.mult, [RW, gfb], [RW])
            rbias = sbt(G, s1, 'f_rbias', [128, 36], F32)
            H.dma(rbias[:], I.rbias_b.ap(), [], [rbias])
            ecap = sbt(G, s1, 'f_ecap', [128, 32], F32)
            H.dma(ecap[:], I.ecap_b.ap(), [], [ecap])
            gfrow = sbt(G, s1, 'f_gfrow', [128, D], F32)
            H.dma(gfrow[:], I.gffn_row_b.ap(), [], [gfrow])
            NB = 4
            ecapa = sbt(G, s1, 'f_ecapa', [128, NT, 32], F32)
            H.dma(ecapa[:], I.ecap_all.ap()[:, 0:NT, :], [], [ecapa])
            hb_all = sbt(G, s1, 'f_hball', [128, NT, D], BF16)
            oh_all = sbt(G, s1, 'f_ohall', [128, 2, NT, 32], F32)
            Abf_all = sbt(G, s1, 'f_Abfall', [128, NT, 32], BF16)
            pos_all = sbt(G, s1, 'f_posall', [128, NT, 32], F32)
            prod_all = sbt(G, s1, 'f_prodall', [128, NT, 32], F32)
            slf_all = sbt(G, s1, 'f_slfall', [128, 2, NT], F32)

            def sm(name, shape, dt=F32):
                return [sbt(G, s1, 'f_%s%d' % (name, i), shape, dt) for i in range(NB)]
            xt, sq, h2T = sm('xt', [128, D]), sm('sq', [128, D]), sm('h2T', [128, 8, 128])
            ss, lg, gmax, goh, ex, gsum, sel = sm('ss', [128, 1]), sm('lg', [128, 36]), sm('gmax', [128, 2]), sm('goh', [128, 4]), \
                sm('ex', [128, 4]), sm('gsum', [128, 2]), sm('sel', [128, 8])
            m12, oh1, oh2, sel2, pp, Af = sm('m12', [128, 4]), sm('oh1', [128, 8]), sm('oh2', [128, 8]), \
                sm('sel2', [128, 8]), sm('pp', [128, 4]), sm('Af', [128, 32])

            def tile_body(i):
                q = i % NB
                x_, hT_, sq_ = xt[q], h2T[q], sq[q]
                bank = ps[q]
                r0 = i * 128
                H.dma(x_[:], Sx.x1[r0:r0 + 128, :], [('x1', r0)], [x_])
                H.memset('pool', ss[q][:], 0.0, [ss[q]])
                P.act(lambda e, o=sq_[:], a=x_[:], acc=ss[q][:]: e.activation(o, a, AF.Square, accum_out=acc), [x_, ss[q]], [sq_, ss[q]])
                H.rsqrt(ss[q][:], ss[q][:], 1.0 / D, EPS, [ss[q]], [ss[q]])
                H.ts('dve', x_[:], x_[:], ss[q][:, 0:1], None, ALU.mult, None, [x_, ss[q]], [x_])
                H.tt('dve', hb_all[:, i, :], x_[:], gfrow[:], ALU.mult, [x_, gfrow], [(hb_all, i)])
                for hf in range(2):
                    for k4 in range(4):
                        kt = hf * 4 + k4
                        H.tr(bank[:, k4 * 128:(k4 + 1) * 128], x_[:, kt * 128:(kt + 1) * 128], identf[:], [x_, identf], [bank])
                    H.cp('act', hT_[:, hf * 4:(hf + 1) * 4, :], bank[:].rearrange("p (a c) -> p a c", a=4), [bank], [(hT_, hf)])
                for kt in range(8):
                    H.mm(bank[:, 0:36], hT_[:, kt, :], RW[:, kt, :], kt == 0, kt == 7, [(hT_, kt // 4), RW], [bank])
                L = lg[q]
                H.tt('dve', L[:], bank[:, 0:36], rbias[:], ALU.add, [bank, rbias], [L])
                gm, go, e_, gs, se = gmax[q], goh[q], ex[q], gsum[q], sel[q]
                H.red('dve', gm[:, 0:1], L[:, 0:4], ALU.max, [L], [gm])
                H.ts('dve', go[:], L[:, 0:4], gm[:, 0:1], None, ALU.is_equal, None, [L, gm], [go])
                H.ts('dve', gm[:, 1:2], gm[:, 0:1], -1.0, None, ALU.mult, None, [gm], [gm])
                H.actf(e_[:], L[:, 0:4], AF.Exp, [L, gm], [e_], bias=gm[:, 1:2])
                H.red('dve', gs[:, 0:1], e_[:], ALU.add, [e_], [gs])
                H.recip(gs[:, 1:2], gs[:, 0:1], [gs], [gs])
                H.ts('dve', se[:], L[:, 4:12], go[:, 0:1], None, ALU.mult, None, [L, go], [se])
                for g in range(1, 4):
                    H.stt('dve', se[:], L[:, 4 + g * 8:12 + g * 8], go[:, g:g + 1], se[:], ALU.mult, ALU.add, [L, go, se], [se])
                mm_, o1_, o2_, s2_, p_ = m12[q], oh1[q], oh2[q], sel2[q], pp[q]
                H.red('dve', mm_[:, 0:1], se[:], ALU.max, [se], [mm_])
                H.ts('dve', o1_[:], se[:], mm_[:, 0:1], None, ALU.is_equal, None, [se, mm_], [o1_])
                H.stt('dve', s2_[:], o1_[:], -1e30, se[:], ALU.mult, ALU.add, [o1_, se], [s2_])
                H.red('dve', mm_[:, 1:2], s2_[:], ALU.max, [s2_], [mm_])
                H.ts('dve', o2_[:], s2_[:], mm_[:, 1:2], None, ALU.is_equal, None, [s2_, mm_], [o2_])
                H.tt('dve', mm_[:, 2:3], mm_[:, 1:2], mm_[:, 0:1], ALU.subtract, [mm_], [mm_])
                H.actf(p_[:, 0:1], mm_[:, 2:3], AF.Exp, [mm_], [p_])
                H.ts('dve', p_[:, 1:2], p_[:, 0:1], 1.0, None, ALU.add, None, [p_], [p_])
                H.recip(p_[:, 2:3], p_[:, 1:2], [p_], [p_])
                H.tt('dve', p_[:, 3:4], p_[:, 0:1], p_[:, 2:3], ALU.mult, [p_], [p_])
                H.tt('dve', cw[0][:, i:i + 1], p_[:, 2:3], gs[:, 1:2], ALU.mult, [p_, gs], [(cw[0], i)])
                H.tt('dve', cw[1][:, i:i + 1], p_[:, 3:4], gs[:, 1:2], ALU.mult, [p_, gs], [(cw[1], i)])
                for k, ok in enumerate((o1_, o2_)):
                    for g in range(4):
                        H.ts('pool', oh_all[:, k, i, g * 8:(g + 1) * 8], ok[:], go[:, g:g + 1], None, ALU.mult, None, [ok, go], [(oh_all, i)])
                H.tt('pool', Af[q][:], oh_all[:, 0, i, :], oh_all[:, 1, i, :], ALU.add, [(oh_all, i)], [Af[q]])
                H.cp('pool', Abf_all[:, i, :], Af[q][:], [Af[q]], [(Abf_all, i)])
                o = bank[:, 64:96]
                H.mm(o, tris[:], Abf_all[:, i, :], True, i == 0, [tris, (Abf_all, i)], [bank])
                for j in range(i):
                    H.mm(o, ones[:], Abf_all[:, j, :], False, j == i - 1, [ones, (Abf_all, j)], [bank])
                pz = pos_all[:, i, :]
                H.ts('dve', pz, o, float(CAP - 1), None, ALU.min, None, [bank], [(pos_all, i)])
                H.tt('dve', pz, pz, ecap[:], ALU.add, [(pos_all, i), ecap], [(pos_all, i)])
                for k in range(2):
                    H.tt('dve', prod_all[:, i, :], oh_all[:, k, i, :], pz, ALU.mult, [(oh_all, i), (pos_all, i)], [(prod_all, i)])
                    H.red('dve', slf_all[:, k, i:i + 1], prod_all[:, i, :], ALU.add, [(prod_all, i)], [(slf_all, k, i)])
                    H.cp('dve', slots[k][:, i:i + 1], slf_all[:, k, i:i + 1], [(slf_all, k, i)], [(slots[k], i)])
                for k in range(2):
                    P.dma(lambda e, k=k, i=i: e.indirect_dma_start(
                        out=Sx.xg.ap(), out_offset=IOA(ap=slots[k][:, i:i + 1], axis=0), in_=hb_all[:, i, :], in_offset=None),
                        [(hb_all, i), (slots[k], i)], ['xg'], q='pool')

            def capture(fn):
                ops = []
                P.add = lambda E, f, r=(), w=(), dma=False: ops.append((E, f, r, w, dma))
                try:
                    fn()
                finally:
                    del P.add
                return ops

            active = []
            nxt = 0
            step = 0
            K_ = None
            while active or nxt < NT:
                if nxt < NT and len(active) < NB and (K_ is None or step % K_ == 0):
                    ops_ = capture(lambda i=nxt: tile_body(i))
                    if K_ is None:
                        K_ = max(1, -(-len(ops_) // NB))
                    active.append([ops_, 0])
                    nxt += 1
                for ch in list(active):
                    P.add(*ch[0][ch[1]])
                    ch[1] += 1
                    if ch[1] >= len(ch[0]):
                        active.remove(ch)
                step += 1
        P.barrier()
        with ExitStack() as s2:
            stg1 = [sbt(G, s2, 'f_stg1%d' % i, [128, 8, 256], F32) for i in range(2)]
            stg3 = [sbt(G, s2, 'f_stg3%d' % i, [128, 8, 256], F32) for i in range(2)]
            stg2 = [sbt(G, s2, 'f_stg2%d' % i, [128, 2, 1024], F32) for i in range(2)]
            W1 = [sbt(G, s2, 'f_W1%d' % i, [128, 8, 256], BF16) for i in range(2)]
            W3 = [sbt(G, s2, 'f_W3%d' % i, [128, 8, 256], BF16) for i in range(2)]
            W2 = [sbt(G, s2, 'f_W2%d' % i, [128, 2, 1024], BF16) for i in range(2)]
            xg = [sbt(G, s2, 'f_xg%d' % i, [128, NSLOT, 1024], BF16) for i in range(2)]
            xgT = [sbt(G, s2, 'f_xgT%d' % i, [128, 8, CAP], BF16) for i in range(2)]
            sa = [sbt(G, s2, 'f_sa%d' % i, [128, CAP], F32) for i in range(2)]
            hid = [sbt(G, s2, 'f_hid%d' % i, [128, 2, CAP], BF16) for i in range(2)]
            yo = [sbt(G, s2, 'f_yo%d' % i, [128, 1024], BF16) for i in range(3)]
            yi = 0
            NE = 32 if 'noF2' not in G.dbg else 0

            def prefetch(e):
                q = e % 2
                H.dma(stg1[q][:], I.w1[e].rearrange("(p k) c -> p k c", k=8), [], [stg1[q]])
                H.dma(stg3[q][:], I.w3[e].rearrange("(p k) c -> p k c", k=8), [], [stg3[q]])
                H.dma(stg2[q][:], I.w2[e].rearrange("(k p) c -> p k c", p=128), [], [stg2[q]])
                H.dma(xg[q][:], Sx.xg[e * CAP:(e + 1) * CAP, :].rearrange("(a p) c -> p a c", p=128), ['xg'], [xg[q]])
                H.cp('dve', W1[q][:], stg1[q][:], [stg1[q]], [W1[q]])
                H.cp('act', W3[q][:], stg3[q][:], [stg3[q]], [W3[q]])
                H.cp('act', W2[q][:], stg2[q][:], [stg2[q]], [W2[q]])

            if NE:
                prefetch(0)
            for e in range(NE):
                q = e % 2
                if e + 1 < NE:
                    prefetch(e + 1)
                for a in range(NSLOT):
                    pb_ = pbk[a % 2]
                    for kt in range(8):
                        H.tr(pb_[:, kt * 128:(kt + 1) * 128], xg[q][:, a, kt:1024:8], ident[:], [xg[q], ident], [pb_])
                    H.cp('act' if a % 2 == 0 else 'dve', xgT[q][:, :, a * 128:(a + 1) * 128], pb_[:].rearrange("p (k t) -> p k t", k=8),
                         [pb_], [(xgT[q], a)])
                xall = [(xgT[q], a) for a in range(NSLOT)]
                for j in range(2):
                    pa, pb2 = ps[0 + 2 * j], ps[1 + 2 * j]
                    for kt in range(8):
                        H.mm(pa[:, 0:CAP], W1[q][:, kt, j * 128:(j + 1) * 128], xgT[q][:, kt, :], kt == 0, kt == 7, [W1[q]] + xall, [pa])
                    for kt in range(8):
                        H.mm(pb2[:, 0:CAP], W3[q][:, kt, j * 128:(j + 1) * 128], xgT[q][:, kt, :], kt == 0, kt == 7, [W3[q]] + xall, [pb2])
                    H.actf(sa[j][:], pa[:, 0:CAP], AF.Silu, [pa], [sa[j]])
                    H.tt('dve', hid[q][:, j, :], sa[j][:], pb2[:, 0:CAP], ALU.mult, [sa[j], pb2], [(hid[q], j)])
                for a in range(NSLOT):
                    y_ = yo[yi % 3]
                    yi += 1
                    for half in range(2):
                        po = ps[4 + half]
                        for j in range(2):
                            H.mm(po[:], hid[q][:, j, a * 128:(a + 1) * 128], W2[q][:, j, half * 512:(half + 1) * 512], j == 0, j == 1,
                                 [(hid[q], j), W2[q]], [po])
                        H.cp('act' if half == 0 else 'dve', y_[:, half * 512:(half + 1) * 512], po[:], [po], [(y_, half)])
                    r0 = e * CAP + a * 128
                    H.dma(Sx.yg[r0:r0 + 128, :], y_[:], [(y_, 0), (y_, 1)], ['yg'], q='pool')
        P.barrier()
        with ExitStack() as s3:
            gfin = sbt(G, s3, 'f_gfin', [128, D], F32)
            H.dma(gfin[:], I.gfin_b.ap(), [], [gfin])
            NB3 = 4
            r1 = [sbt(G, s3, 'f_r1%d' % i, [128, D], BF16) for i in range(NB3)]
            r2 = [sbt(G, s3, 'f_r2%d' % i, [128, D], BF16) for i in range(NB3)]
            xt = [sbt(G, s3, 'f_x3%d' % i, [128, D], F32) for i in range(NB3)]
            sq = [sbt(G, s3, 'f_sq3%d' % i, [128, D], BF16) for i in range(NB3)]
            ss = [sbt(G, s3, 'f_ss3%d' % i, [128, 1], F32) for i in range(NB3)]

            def f3_body(i):
                q = i % NB3
                r0 = i * 128
                H.dma(xt[q][:], Sx.x1[r0:r0 + 128, :], [('x1', r0)], [xt[q]])
                for k, rr_ in enumerate((r1[q], r2[q])):
                    P.dma(lambda e, k=k, i=i, rr_=rr_: e.indirect_dma_start(
                        out=rr_[:], out_offset=None, in_=Sx.yg.ap(), in_offset=IOA(ap=slots[k][:, i:i + 1], axis=0)),
                        ['yg', (slots[k], i)], [rr_], q='pool')
                H.memset('pool', ss[q][:], 0.0, [ss[q]])
                H.stt('dve', xt[q][:], r1[q][:], cw[0][:, i:i + 1], xt[q][:], ALU.mult, ALU.add, [r1[q], (cw[0], i), xt[q]], [xt[q]])
                H.stt('dve', xt[q][:], r2[q][:], cw[1][:, i:i + 1], xt[q][:], ALU.mult, ALU.add, [r2[q], (cw[1], i), xt[q]], [xt[q]])
                P.act(lambda e, o=sq[q][:], a=xt[q][:], acc=ss[q][:]: e.activation(o, a, AF.Square, accum_out=acc), [xt[q], ss[q]], [sq[q], ss[q]])
                H.rsqrt(ss[q][:], ss[q][:], 1.0 / D, EPS, [ss[q]], [ss[q]])
                H.stt('dve', xt[q][:], xt[q][:], ss[q][:, 0:1], gfin[:], ALU.mult, ALU.mult, [xt[q], ss[q], gfin], [xt[q]])
                H.dma(G.out[r0:r0 + 128, :], xt[q][:], [xt[q]], [('out', i)])

            def capture_f3(fn):
                ops = []
                P.add = lambda E, f, r=(), w=(), dma=False: ops.append((E, f, r, w, dma))
                try:
                    fn()
                finally:
                    del P.add
                return ops

            NT3 = NT if 'noF3' not in G.dbg else 0
            active = []
            nxt = 0
            step = 0
            while active or nxt < NT3:
                if nxt < NT3 and len(active) < NB3 and step % 3 == 0:
                    active.append([capture_f3(lambda i=nxt: f3_body(i)), 0])
                    nxt += 1
                for ch in list(active):
                    P.add(*ch[0][ch[1]])
                    ch[1] += 1
                    if ch[1] >= len(ch[0]):
                        active.remove(ch)
                step += 1


def host_prep(inp):
    f32 = np.float32
    bf = ml_dtypes.bfloat16
    out = {}
    w_in = np.asarray(inp['w_in'][0], f32)
    ext = np.zeros((D, NW), f32)
    ext[:, :4640] = w_in
    kr = w_in[:, 640:672]
    ext[:, KA_OFF + 64:KA_OFF + 96] = kr
    ext[:, KB_OFF + 64:KB_OFF + 80] = kr[:, 16:32]
    ext[:, KB_OFF + 80:KB_OFF + 96] = kr[:, 0:16]
    out['w_in_ext'] = ext
    out['g_mix'] = np.ascontiguousarray(np.asarray(inp['norm_mix_g'][0], f32).reshape(8, 128).T)
    wuq = np.asarray(inp['w_uq'][0], f32).reshape(384, 8, 96)
    e = np.zeros((384, 2, 8, 128), f32)
    e[:, 0, :, 0:96] = wuq
    e[:, 1, :, 64:80] = wuq[:, :, 80:96]
    e[:, 1, :, 80:96] = wuq[:, :, 64:80]
    out['wuq_ext'] = e.reshape(384, 2048)
    out['g_q'] = np.ascontiguousarray(np.asarray(inp['q_norm_g'][0], f32).reshape(3, 128).T)
    wukv = np.asarray(inp['w_ukv'][0], f32).reshape(256, 8, 128)
    e = np.zeros((256, 2, 8, 128), f32)
    e[:, 0, :, 0:64] = wukv[:, :, 0:64]
    for h in range(8):
        o = (h % 2) * 64
        e[:, 1, h, o:o + 64] = wukv[:, h, 64:128]
    out['wukv_ext'] = e.reshape(256, 2048)
    out['g_kv'] = np.ascontiguousarray(np.asarray(inp['kv_norm_g'][0], f32).reshape(2, 128).T)
    pos = np.arange(S, dtype=np.float32)
    inv_freq = (10000.0 ** (-np.arange(0, 32, 2, dtype=np.float32) / 32)).astype(np.float32)
    ang = pos[None, :] * inv_freq[:, None]
    cos, sin = np.cos(ang).astype(f32), np.sin(ang).astype(f32)
    c128 = np.ones((128, S), f32)
    s128 = np.zeros((128, S), f32)
    c128[64:80] = cos
    c128[80:96] = cos
    s128[64:80] = -sin
    s128[80:96] = sin
    out['cos128'] = c128
    out['sin128'] = s128
    out['ident_bf'] = np.eye(128, dtype=f32).astype(bf)
    out['ones_bf'] = np.ones((128, 128), f32).astype(bf)
    op = np.zeros((2, 128, 128), f32)
    op[0, :, 0:64] = 1
    op[1, :, 64:128] = 1
    out['onespad_bf'] = op.astype(bf)
    def cols(v, n):
        return np.asarray(v, f32).reshape(n, 128).T
    cst = np.zeros((128, 64), f32)
    cst[:, 0:15] = cols(inp['mu_prev'][0], 15)
    cst[:, 15:30] = cols(inp['mu_next'][0], 15)
    cst[:, 30:34] = cols(inp['k_k'][0], 4)
    cst[:, 34:38] = cols(inp['k_a'][0], 4)
    cst[:, 38:42] = cols(inp['r_k'][0], 4)
    cst[:, 42:46] = cols(inp['lnx_g'][0], 4)
    cst[:, 46:50] = cols(inp['lnx_b'][0], 4)
    a0 = np.asarray(inp['a0'][0], f32)
    cst[:, 50:54] = cols(a0[0], 4)
    cst[:, 54:58] = cols(a0[1], 4)
    out['rw_cst'] = cst
    out['w_up2'] = np.ascontiguousarray(np.asarray(inp['w_up'][0], f32).reshape(128, 512))
    out['a_up2'] = np.ascontiguousarray(np.asarray(inp['a_up'][0], f32).reshape(128, 512))
    out['g_up2'] = np.ascontiguousarray(np.asarray(inp['g_up'][0], f32))
    w0 = np.asarray(inp['w0'][0], f32)
    out['w0b'] = np.ascontiguousarray(np.broadcast_to(w0[:, None, :], (2, 128, 512)))
    idx = np.arange(128)
    same = (idx[:, None] // 64) == (idx[None, :] // 64)
    sp, tp = idx[:, None] % 64, idx[None, :] % 64
    cf = -np.exp(-0.5)
    tri3 = np.zeros((2, 128, 384), f32)
    tri3[0, :, 0:128] = same & (sp <= tp)
    tri3[0, :, 128:256] = same & (sp < tp)
    tri3[0, :, 256:384] = same & (sp > tp)
    tri3[1, :, 0:128] = same & (sp >= tp)
    tri3[1, :, 128:256] = same & (sp > tp)
    tri3[1, :, 256:384] = same & (sp < tp)
    out['tri3'] = (tri3 * cf).astype(f32)
    mT = np.zeros((2, 128, 2, 256), f32)
    mL = np.zeros((2, 128, 4, 128), f32)
    for a in range(2):
        mT[0, :, a, 0:128] = same & (tp > sp)
        mT[0, :, a, 128:256] = same & (tp >= sp)
        mT[1, :, a, 0:128] = same & (tp < sp)
        mT[1, :, a, 128:256] = same & (tp <= sp)
    for a in range(4):
        mL[0, :, a, :] = same & (tp < sp)
        mL[1, :, a, :] = same & (tp > sp)
    out['maskT'] = mT.reshape(2, 128, 512).astype(bf)
    out['maskL'] = mL.reshape(2, 128, 512).astype(bf)
    out['ident4'] = np.tile(np.eye(128, dtype=f32), (1, 4)).astype(bf)
    out['bdones_bf'] = same.astype(f32).astype(bf)
    out['bdones_f'] = same.astype(f32)
    out['w_br_mla'] = np.ascontiguousarray(np.asarray(inp['w_br_mla'][0], f32))
    out['w_br_rwkv'] = np.ascontiguousarray(np.asarray(inp['w_br_rwkv'][0], f32))
    out['w_out'] = np.ascontiguousarray(np.asarray(inp['w_out'][0], f32))
    gbv = np.asarray(inp['gate_b'][0], f32)
    out['gate_b2'] = np.ascontiguousarray(gbv.reshape(16, 128).T)
    gf = cols(inp['norm_ffn_g'][0], 8)
    out['gffn_b'] = np.ascontiguousarray(np.broadcast_to(gf[:, :, None], (128, 8, 256)))
    out['gffn_row_b'] = np.ascontiguousarray(np.broadcast_to(np.asarray(inp['norm_ffn_g'][0], f32)[None, :], (128, 1024)))
    out['ident_f'] = np.eye(128, dtype=f32)
    out['tris_bf'] = (idx[:, None] < idx[None, :]).astype(f32).astype(bf)
    rwc = np.concatenate([np.asarray(inp['router_group_w'][0], f32), np.asarray(inp['router_expert_w'][0], f32)], axis=1)
    out['rw_router'] = np.ascontiguousarray(rwc.reshape(8, 128, 36).transpose(1, 0, 2))
    rb = np.concatenate([np.asarray(inp['router_group_b'][0], f32), np.asarray(inp['router_expert_b'][0], f32)])
    out['rbias_b'] = np.ascontiguousarray(np.broadcast_to(rb[None, :], (128, 36)))
    out['ecap_b'] = np.ascontiguousarray(np.broadcast_to((np.arange(32, dtype=f32) * CAP)[None, :], (128, 32)))
    out['ecap_all'] = np.ascontiguousarray(np.broadcast_to((np.arange(32, dtype=f32) * CAP)[None, None, :], (128, 32, 32)))
    out['w1'] = np.ascontiguousarray(np.asarray(inp['w1'][0], f32))
    out['w3'] = np.ascontiguousarray(np.asarray(inp['w3'][0], f32))
    out['w2'] = np.ascontiguousarray(np.asarray(inp['w2'][0], f32))
    out['gfin_b'] = np.ascontiguousarray(np.broadcast_to(np.asarray(inp['norm_final_g'], f32)[None, :], (128, 1024)))
    return out


_NC_CACHE = {}


def kernel(**inputs):
    n = 8
    hp = host_prep(inputs)
    x = np.ascontiguousarray(np.asarray(inputs['x'], np.float32)).reshape(16 * S, D)
    if 'nc' not in _NC_CACHE:
        _NC_CACHE['nc'] = build_nc(nseq=NSEQ_CORE)
    nc = _NC_CACHE['nc']
    rows = NSEQ_CORE * S
    in_maps = []
    for c in range(n):
        m = dict(hp)
        m['x'] = np.ascontiguousarray(x[c * rows:(c + 1) * rows])
        in_maps.append(m)
    res = run_bass_kernel_spmd(nc, in_maps, core_ids=list(range(n)))
    outs = [np.asarray(r['out'], np.float32) for r in res.results]
    return np.concatenate(outs, axis=0).reshape(16, S, D)
```

```python
import numpy as np
import concourse.bass as bass
import concourse.mybir as mybir
from contextlib import ExitStack

F32 = mybir.dt.float32
BF16 = mybir.dt.bfloat16
I32 = mybir.dt.int32
AF = mybir.ActivationFunctionType
ALU = mybir.AluOpType
AX = mybir.AxisListType

ENGS = ['pe', 'act', 'dve', 'pool', 'sp']
CENGS = ['pe', 'act', 'dve', 'pool']
WINDOW = 10 ** 9


class Prog:
    def __init__(self, nc, stack):
        self.nc = nc
        self.stack = stack
        self.ops = {e: [] for e in ENGS}
        self.count = {e: 0 for e in CENGS}
        self.known = {e: {f: 0 for f in CENGS} for e in ENGS}
        self.dknown = {e: {} for e in ENGS}
        self.clock = {e: [None] for e in CENGS}
        self.sem = {e: stack.enter_context(nc.semaphore('s_' + e)) for e in CENGS}
        self.NDMA = 16
        self.dsem = {q: [stack.enter_context(nc.semaphore('d_%s%d' % (q, i))) for i in range(self.NDMA)]
                     for q in ['sp', 'pool']}
        self.dcount = {'sp': 0, 'pool': 0}
        self.lastw = {}
        self.readers = {}
        self.nwaits = 0

    def _merge(self, E, kn, dk):
        k = self.known[E]
        for f, v in kn.items():
            if v > k[f]:
                k[f] = v
        d = self.dknown[E]
        for f, v in dk.items():
            if v > d.get(f, 0):
                d[f] = v

    def _need(self, E, ev, waits):
        if ev[0] == 'c':
            _, F, n = ev
            if F == E:
                if E == 'pe':
                    return
                if n <= self.known[E][E] or n < self.count[E] - WINDOW + 1:
                    return
            elif self.known[E][F] >= n:
                return
            waits[('c', F)] = max(waits.get(('c', F), 0), n)
            kn, dk = self.clock[F][n]
            self._merge(E, kn, dk)
        else:
            _, q, k, target, kn, dk = ev
            if self.dknown[E].get((q, k), 0) >= target:
                return
            waits[('d', q, k)] = max(waits.get(('d', q, k), 0), target)
            self.dknown[E][(q, k)] = target
            self._merge(E, kn, dk)

    def add(self, E, fn, r=(), w=(), dma=False):
        waits = {}
        deps = []
        for res in r:
            ev = self.lastw.get(res)
            if ev is not None:
                deps.append(ev)
        for res in w:
            ev = self.lastw.get(res)
            if ev is not None:
                deps.append(ev)
            rd = self.readers.get(res)
            if rd:
                deps.extend(rd.values())
        if dma:
            j = self.dcount[E]
            k = j % self.NDMA
            target = 16 * (j // self.NDMA + 1)
            if j >= self.NDMA:
                waits[('d', E, k)] = target - 16
                self.dknown[E][(E, k)] = target - 16
            self.dcount[E] += 1
        for ev in deps:
            self._need(E, ev, waits)
        wl = []
        for key, v in waits.items():
            if key[0] == 'c':
                wl.append((self.sem[key[1]], v))
            else:
                wl.append((self.dsem[key[1]][key[2]], v))
        self.nwaits += len(wl)
        if dma:
            myev = ('d', E, k, target, dict(self.known[E]), dict(self.dknown[E]))
            self.ops[E].append((wl, fn, self.dsem[E][k], 16))
            rkey = ('d', E, k)
        else:
            self.count[E] += 1
            n = self.count[E]
            kn = dict(self.known[E])
            kn[E] = n
            self.clock[E].append((kn, dict(self.dknown[E])))
            myev = ('c', E, n)
            self.ops[E].append((wl, fn, self.sem[E], 1))
            rkey = ('c', E)
        for res in r:
            self.readers.setdefault(res, {})[rkey] = myev
        for res in w:
            self.lastw[res] = myev
            self.readers[res] = {}
        return myev

    def pe(self, fn, r=(), w=()):
        return self.add('pe', fn, r, w)

    def act(self, fn, r=(), w=()):
        return self.add('act', fn, r, w)

    def dve(self, fn, r=(), w=()):
        return self.add('dve', fn, r, w)

    def pool(self, fn, r=(), w=()):
        return self.add('pool', fn, r, w)

    def dma(self, fn, r=(), w=(), q='sp'):
        return self.add(q, fn, r, w, dma=True)

    def barrier(self):
        for E in ENGS:
            wl = []
            for F in CENGS:
                n = self.count[F]
                if n > self.known[E][F] and n > 0:
                    wl.append((self.sem[F], n))
                    self.known[E][F] = n
            for q in ['sp', 'pool']:
                j = self.dcount[q]
                for k in range(min(j, self.NDMA)):
                    cnt = (j - 1 - k) // self.NDMA + 1
                    t = 16 * cnt
                    if self.dknown[E].get((q, k), 0) < t:
                        wl.append((self.dsem[q][k], t))
                        self.dknown[E][(q, k)] = t
            self.ops[E].append((wl, None, None, 0))
            self.nwaits += len(wl)
        self.lastw = {}
        self.readers = {}

    def emit(self):
        nc = self.nc
        self.barrier()
        ops = self.ops

        def run(E, eng):
            for wl, fn, sem, inc in ops[E]:
                for s, v in wl:
                    eng.wait_ge(s, v)
                if fn is not None:
                    fn(eng).then_inc(sem, inc)

        with nc.Block() as block:
            @block.tensor
            def _(e):
                run('pe', e)

            @block.scalar
            def _(e):
                run('act', e)

            @block.vector
            def _(e):
                run('dve', e)

            @block.gpsimd
            def _(e):
                run('pool', e)

            @block.sync
            def _(e):
                run('sp', e)

from concourse.bass_utils import run_bass_kernel_spmd
import ml_dtypes

S = 2048
D = 1024
NW = 4640 + 256
KA_OFF = 4640
KB_OFF = 4768
NSEQ_CORE = 2
EPS = 1e-6
ATT_SCALE = 96 ** -0.5


class T:
    def __init__(self, h, name):
        self.h = h
        self.name = name

    def __getitem__(self, k):
        return self.h[k]


class Ctx:
    pass


def ops_helpers(P):
    H = Ctx()

    def mm(out, lhsT, rhs, start, stop, r, w):
        P.pe(lambda e: e.matmul(out, lhsT, rhs, start=start, stop=stop), r, w)

    def tr(out, in_, ident, r, w):
        P.pe(lambda e: e.transpose(out, in_, ident), r, w)

    def cp(eng, out, in_, r, w):
        if eng == 'act':
            P.act(lambda e: e.copy(out, in_), r, w)
        else:
            P.add(eng, lambda e: e.tensor_copy(out, in_), r, w)

    def tt(eng, out, a, b, op, r, w):
        P.add(eng, lambda e: e.tensor_tensor(out, a, b, op), r, w)

    def ts(eng, out, a, s1, s2, op0, op1, r, w):
        if op1 is None:
            P.add(eng, lambda e: e.tensor_scalar(out, a, s1, None, op0), r, w)
        else:
            P.add(eng, lambda e: e.tensor_scalar(out, a, s1, s2, op0, op1), r, w)

    def stt(eng, out, in0, scalar, in1, op0, op1, r, w):
        eng = 'dve'
        P.add(eng, lambda e: e.scalar_tensor_tensor(out, in0, scalar, in1, op0, op1), r, w)

    def actf(out, in_, func, r, w, bias=None, scale=None):
        kw = {}
        if bias is not None:
            kw['bias'] = bias
        if scale is not None:
            kw['scale'] = scale
        P.act(lambda e: e.activation(out, in_, func, **kw), r, w)

    def red(eng, out, in_, op, r, w):
        P.add(eng, lambda e: e.tensor_reduce(out, in_, AX.X, op), r, w)

    def recip(out, in_, r, w):
        P.dve(lambda e: e.reciprocal(out, in_), r, w)

    def rsqrt(out, in_, scale, bias, r, w):
        actf(out, in_, AF.Sqrt, r, w, bias=bias, scale=scale)
        recip(out, out, w, w)

    H.rsqrt = rsqrt

    def rsqrt_act(out, in_, scale, bias, r, w):
        actf(out, in_, AF.Ln, r, w, bias=bias, scale=scale)
        actf(out, out, AF.Exp, w, w, scale=-0.5)

    H.rsqrt_act = rsqrt_act

    def dma(out, in_, r, w, q='sp'):
        P.dma(lambda e: e.dma_start(out=out, in_=in_), r, w, q=q)

    def memset(eng, ap, val, w):
        P.add(eng, lambda e: e.memset(ap, val), (), w)

    H.mm, H.tr, H.cp, H.tt, H.ts, H.stt, H.actf, H.red, H.recip, H.dma, H.memset = \
        mm, tr, cp, tt, ts, stt, actf, red, recip, dma, memset
    return H


def build_nc(nseq=NSEQ_CORE, phases=('0', 'A', 'C', 'D', 'E', 'F'), dbg=()):
    nc = bass.Bass("TRN2", target_bir_lowering=False)
    G = Ctx()
    G.nc = nc
    G.nseq = nseq
    G.dbg = dbg

    def din(name, shape, dt=F32):
        return nc.dram_tensor(name, list(shape), dt, kind="ExternalInput")

    def dscr(name, shape, dt):
        kind = "ExternalOutput" if name in dbg else "Internal"
        return nc.dram_tensor(name, list(shape), dt, kind=kind)

    I = Ctx()
    G.I = I
    I.x = din('x', [nseq * S, D])
    I.w_in = din('w_in_ext', [D, NW])
    I.g_mix = din('g_mix', [128, 8])
    I.wuq = din('wuq_ext', [384, 2048])
    I.g_q = din('g_q', [128, 3])
    I.wukv = din('wukv_ext', [256, 2048])
    I.g_kv = din('g_kv', [128, 2])
    I.cos128 = din('cos128', [128, S])
    I.sin128 = din('sin128', [128, S])
    I.ident = din('ident_bf', [128, 128], BF16)
    I.ones = din('ones_bf', [128, 128], BF16)
    I.onespad = din('onespad_bf', [2, 128, 128], BF16)
    I.rw_cst = din('rw_cst', [128, 64])
    I.w_up = din('w_up2', [128, 512])
    I.a_up = din('a_up2', [128, 512])
    I.g_up = din('g_up2', [128, 512])
    I.w0b = din('w0b', [2, 128, 512])
    I.tri3 = din('tri3', [2, 128, 384])
    I.maskT = din('maskT', [2, 128, 512], BF16)
    I.maskL = din('maskL', [2, 128, 512], BF16)
    I.ident4 = din('ident4', [128, 512], BF16)
    I.bdones = din('bdones_bf', [128, 128], BF16)
    I.bdones_f = din('bdones_f', [128, 128])
    I.w_br_mla = din('w_br_mla', [512, 1024])
    I.w_br_rwkv = din('w_br_rwkv', [512, 1024])
    I.w_out = din('w_out', [1024, 1024])
    I.gate_b = din('gate_b2', [128, 16])
    I.gffn_b = din('gffn_b', [128, 8, 256])
    I.gffn_row_b = din('gffn_row_b', [128, 1024])
    I.ident_f = din('ident_f', [128, 128])
    I.tris = din('tris_bf', [128, 128], BF16)
    I.rw_router = din('rw_router', [128, 8, 36])
    I.rbias_b = din('rbias_b', [128, 36])
    I.ecap_b = din('ecap_b', [128, 32])
    I.ecap_all = din('ecap_all', [128, 32, 32])
    I.w1 = din('w1', [32, 1024, 256])
    I.w3 = din('w3', [32, 1024, 256])
    I.w2 = din('w2', [32, 256, 1024])
    I.gfin_b = din('gfin_b', [128, 1024])

    Sx = Ctx()
    G.S = Sx
    Sx.Wbf = dscr('Wbf', [8, 128, NW], BF16)
    Sx.hT = dscr('hT_d', [nseq, 8, 128, S], BF16)
    Sx.o1 = dscr('o1_d', [nseq, 4, 128, S], BF16)
    Sx.o2 = dscr('o2_d', [nseq, 4, 128, S], BF16)
    Sx.rw = dscr('rw_d', [nseq, 4, 4, 128, S], BF16)
    Sx.x1 = dscr('x1_d', [nseq * S, D], F32)
    Sx.wbm = dscr('wbm_d', [4, 128, 1024], BF16)
    Sx.wbr = dscr('wbr_d', [4, 128, 1024], BF16)
    Sx.wout = dscr('wout_d', [8, 128, 1024], BF16)
    Sx.xg = dscr('xg_d', [32 * CAP, D], BF16)
    Sx.yg = dscr('yg_d', [32 * CAP, D], BF16)
    G.out = nc.dram_tensor('out', [nseq * S, D], F32, kind="ExternalOutput")

    with ExitStack() as stack:
        P = Prog(nc, stack)
        G.P = P
        G.H = ops_helpers(P)
        G.ps = [T(stack.enter_context(nc.psum_tensor('ps%d' % i, [128, 512], F32)), 'ps%d' % i) for i in range(6)]
        G.pb = [T(stack.enter_context(nc.psum_tensor('pb%d' % i, [128, 1024], BF16)), 'pb%d' % i) for i in range(2)]
        if '0' in phases:
            phase0(G)
            P.barrier()
        for b in range(nseq):
            if 'A' in phases:
                phaseA(G, b)
                P.barrier()
            if 'C' in phases:
                phaseC(G, b)
                P.barrier()
            if 'D' in phases:
                phaseD(G, b)
                P.barrier()
            if 'E' in phases:
                phaseE(G, b)
                P.barrier()
        if 'F' in phases:
            phaseF(G)
            P.barrier()
        if not phases or 'F' not in phases:
            with ExitStack() as st:
                z = T(st.enter_context(nc.sbuf_tensor('zz', [128, D], F32)), 'zz')
                G.H.memset('dve', z[:], 0.0, [z])
                G.H.dma(G.out[0:128, :], z[:], [z], ['out'])
                P.barrier()
        P.emit()
    return nc


_UID = [0]


def sbt(G, st, name, shape, dt):
    _UID[0] += 1
    name = '%s_u%d' % (name, _UID[0])
    return T(st.enter_context(G.nc.sbuf_tensor(name, list(shape), dt)), name)


def phase0(G):
    H, I, Sx = G.H, G.I, G.S
    with ExitStack() as st:
        g = sbt(G, st, 'p0_g', [128, 8], F32)
        H.dma(g[:], I.g_mix.ap(), [], [g])
        stg = [sbt(G, st, 'p0_stg%d' % i, [128, NW], F32) for i in range(2)]
        wb = [sbt(G, st, 'p0_wb%d' % i, [128, NW], BF16) for i in range(2)]
        def load(kt):
            H.dma(stg[kt % 2][:], I.w_in[kt * 128:(kt + 1) * 128, :], [], [stg[kt % 2]])
        load(0)
        load(1)
        hw = NW // 2
        for kt in range(8):
            s_, w_ = stg[kt % 2], wb[kt % 2]
            H.ts('dve', w_[:, 0:hw], s_[:, 0:hw], g[:, kt:kt + 1], None, ALU.mult, None, [s_, g], [(w_, 0)])
            H.actf(w_[:, hw:NW], s_[:, hw:NW], AF.Copy, [s_, g], [(w_, 1)], scale=g[:, kt:kt + 1])
            if kt + 2 < 8:
                load(kt + 2)
            H.dma(Sx.Wbf[kt], w_[:], [(w_, 0), (w_, 1)], [('Wbf', kt)])
        jobs = [(I.w_br_mla, Sx.wbm, 0, 4, 'wbm'), (I.w_br_rwkv, Sx.wbr, 0, 4, 'wbr'), (I.w_out, Sx.wout, 0, 4, 'wout0'), (I.w_out, Sx.wout, 4, 4, 'wout1')]
        for ji, (src, dst, k0, nk, nm) in enumerate(jobs):
            s_, w_ = stg[ji % 2], wb[ji % 2]
            H.dma(s_[:, 0:nk * 1024].rearrange("p (k c) -> p k c", k=nk), src[k0 * 128:(k0 + nk) * 128, :].rearrange("(k p) c -> p k c", p=128),
                  [], [s_])
            H.cp('dve', w_[:, 0:2048], s_[:, 0:2048], [s_], [(w_, 0)])
            H.cp('act', w_[:, 2048:4096], s_[:, 2048:4096], [s_], [(w_, 1)])
            H.dma(dst.ap()[k0:k0 + nk].rearrange("k p c -> p k c"), w_[:, 0:nk * 1024].rearrange("p (k c) -> p k c", k=nk),
                  [(w_, 0), (w_, 1)], [(nm,)])


def phaseA(G, b):
    H, I, Sx = G.H, G.I, G.S
    with ExitStack() as st:
        ident = sbt(G, st, 'a_ident', [128, 128], BF16)
        H.dma(ident[:], I.ident.ap(), [], [ident])
        hT = sbt(G, st, 'a_hT', [128, 8, S], BF16)
        xt = [sbt(G, st, 'a_xt%d' % i, [128, D], F32) for i in range(2)]
        sq = sbt(G, st, 'a_sq', [128, D], F32)
        ss = [sbt(G, st, 'a_ss%d' % i, [128, 1], F32) for i in range(2)]
        xn = [sbt(G, st, 'a_xn%d' % i, [128, D], BF16) for i in range(2)]
        for i in range(16):
            x_, s_, n_ = xt[i % 2], ss[i % 2], xn[i % 2]
            pb = G.pb[i % 2]
            r0 = (b * 16 + i) * 128
            H.dma(x_[:], I.x[r0:r0 + 128, :], [], [x_], q='sp' if i % 2 == 0 else 'pool')
            H.memset('pool', s_[:], 0.0, [s_])
            P_ = G.P
            P_.act(lambda e, o=sq[:], a=x_[:], acc=s_[:]: e.activation(o, a, AF.Square, accum_out=acc), [x_, s_], [sq, s_])
            H.rsqrt(s_[:], s_[:], 1.0 / D, EPS, [s_], [s_])
            H.ts('dve', n_[:], x_[:], s_[:, 0:1], None, ALU.mult, None, [x_, s_], [n_])
            for kt in range(8):
                H.tr(pb[:, kt * 128:(kt + 1) * 128], n_[:, kt * 128:(kt + 1) * 128], ident[:], [n_, ident], [pb])
            H.cp('act', hT[:, :, i * 128:(i + 1) * 128], pb[:].rearrange("p (k t) -> p k t", k=8), [pb], [(hT, i)])
        for kt in range(8):
            H.dma(Sx.hT[b, kt], hT[:, kt, :], [(hT, i) for i in range(16)], [('hT_d', b, kt)],
                  q='sp' if kt % 2 == 0 else 'pool')


def phaseC(G, b):
    H, I, Sx = G.H, G.I, G.S
    ps = G.ps
    with ExitStack() as st:
        hT = sbt(G, st, 'c_hT', [128, 8, S], BF16)
        for kt in range(8):
            H.dma(hT[:, kt, :], Sx.hT[b, kt], [('hT_d', b, kt)], [(hT, kt)])
        hT_all = [(hT, kt) for kt in range(8)]
        Wm = sbt(G, st, 'c_Wm', [128, 8, 896], BF16)
        H.dma(Wm[:, :, 0:640], Sx.Wbf.ap()[:, :, 0:640].rearrange("k p c -> p k c"), [('Wbf', k) for k in range(8)], [(Wm, 0)])
        H.dma(Wm[:, :, 640:896], Sx.Wbf.ap()[:, :, KA_OFF:NW].rearrange("k p c -> p k c"), [('Wbf', k) for k in range(8)], [(Wm, 1)])
        Wm_all = [(Wm, 0), (Wm, 1)]
        gq = sbt(G, st, 'c_gq', [128, 3], F32)
        gkv = sbt(G, st, 'c_gkv', [128, 2], F32)
        H.dma(gq[:], I.g_q.ap(), [], [gq])
        H.dma(gkv[:], I.g_kv.ap(), [], [gkv])
        wuq = sbt(G, st, 'c_wuq', [128, 3, 2048], BF16)
        wukv = sbt(G, st, 'c_wukv', [128, 2, 2048], BF16)
        stg = sbt(G, st, 'c_stg', [128, 2048], F32)
        for j in range(3):
            H.dma(stg[:], I.wuq[j * 128:(j + 1) * 128, :], [], [stg])
            H.ts('dve', wuq[:, j, :], stg[:], gq[:, j:j + 1], None, ALU.mult, None, [stg, gq], [wuq])
        for j in range(2):
            H.dma(stg[:], I.wukv[j * 128:(j + 1) * 128, :], [], [stg])
            H.ts('dve', wukv[:, j, :], stg[:], gkv[:, j:j + 1], None, ALU.mult, None, [stg, gkv], [wukv])
        cosT = sbt(G, st, 'c_cos', [128, S], F32)
        sinT = sbt(G, st, 'c_sin', [128, S], F32)
        H.dma(cosT[:], I.cos128.ap(), [], [cosT])
        H.dma(sinT[:], I.sin128.ap(), [], [sinT])
        ones = sbt(G, st, 'c_ones', [128, 128], BF16)
        H.dma(ones[:], I.ones.ap(), [], [ones])
        onespad = sbt(G, st, 'c_onespad', [128, 2, 128], BF16)
        H.dma(onespad[:], I.onespad.ap().rearrange("h p c -> p h c"), [], [onespad])
        cq = sbt(G, st, 'c_cq', [128, 3, S], BF16)
        ckv = sbt(G, st, 'c_ckv', [128, 2, S], BF16)
        o1T = sbt(G, st, 'c_o1T', [128, 4, S], BF16)
        zsb = sbt(G, st, 'c_zsb', [128, 3, 512], F32)
        sqb = sbt(G, st, 'c_sqb', [128, 3, 512], BF16)
        rr = sbt(G, st, 'c_rr', [128, 512], F32)
        pi = 0
        for tb in range(4):
            tsl = slice(tb * 512, (tb + 1) * 512)
            for (c0, ntile, n, dst) in ((0, 3, 384, cq), (384, 2, 256, ckv)):
                for j in range(ntile):
                    p_ = ps[pi % 4]
                    pi += 1
                    for kt in range(8):
                        H.mm(p_[:], Wm[:, kt, c0 + j * 128:c0 + (j + 1) * 128], hT[:, kt, tsl], kt == 0, kt == 7,
                             [(Wm, 0), (hT, kt)], [p_])
                    H.cp('act', zsb[:, j, :], p_[:], [p_], [(zsb, j)])
                    H.actf(sqb[:, j, :], zsb[:, j, :], AF.Square, [(zsb, j)], [(sqb, j)])
                p2 = ps[4 + (pi % 2)]
                for j in range(ntile):
                    H.mm(p2[:], ones[:], sqb[:, j, :], j == 0, j == ntile - 1, [ones, (sqb, j)], [p2])
                H.rsqrt_act(rr[:], p2[:], 1.0 / n, EPS, [p2], [rr])
                for j in range(ntile):
                    H.tt('dve', dst[:, j, tsl], zsb[:, j, :], rr[:], ALU.mult,
                         [(zsb, j), rr], [(dst, tb)])
        cq_all = [(cq, tb) for tb in range(4)]
        ckv_all = [(ckv, tb) for tb in range(4)]
        QT = [sbt(G, st, 'c_QT%d' % i, [128, S], BF16) for i in range(2)]
        KT = [sbt(G, st, 'c_KT%d' % i, [128, S], BF16) for i in range(2)]
        Vp = [sbt(G, st, 'c_Vp%d' % i, [128, 16, 128], BF16) for i in range(2)]
        krot = sbt(G, st, 'c_krot', [128, S], BF16)
        t1 = [sbt(G, st, 'c_t1%d' % i, [128, 512], F32) for i in range(2)]
        t2 = [sbt(G, st, 'c_t2%d' % i, [128, 512], F32) for i in range(2)]
        PT = [sbt(G, st, 'c_PT%d' % i, [128, 512], BF16) for i in range(4)]
        rs = [sbt(G, st, 'c_rs%d' % i, [128, 512], F32) for i in range(2)]
        H.memset('pool', Vp[0][:, :, 64:128], 1.0, [(Vp[0], 'ones')])
        H.memset('pool', Vp[1][:, :, 0:64], 1.0, [(Vp[1], 'ones')])
        cnt = {'ti': 0, 'pt': 0}
        for tb in range(4):
            tsl = slice(tb * 512, (tb + 1) * 512)
            pA, pB = ps[0], ps[1]
            for kt in range(8):
                H.mm(pA[:], Wm[:, kt, 640:768], hT[:, kt, tsl], kt == 0, kt == 7, [(Wm, 1), (hT, kt)], [pA])
            for kt in range(8):
                H.mm(pB[:], Wm[:, kt, 768:896], hT[:, kt, tsl], kt == 0, kt == 7, [(Wm, 1), (hT, kt)], [pB])
            a_, b_ = t1[cnt['ti'] % 2], t2[cnt['ti'] % 2]
            cnt['ti'] += 1
            H.tt('dve', a_[:], pA[:], cosT[:, tsl], ALU.mult, [pA, cosT], [a_])
            H.tt('dve', b_[:], pB[:], sinT[:, tsl], ALU.mult, [pB, sinT], [b_])
            H.tt('dve', krot[:, tsl], a_[:], b_[:], ALU.add, [a_, b_], [(krot, tb)])
        krot_all = [(krot, tb) for tb in range(4)]

        def prep_head(h):
            Q_, K_, V_ = QT[h % 2], KT[h % 2], Vp[h % 2]
            for tb in range(4):
                tsl = slice(tb * 512, (tb + 1) * 512)
                pA, pB = ps[0], ps[1]
                for j in range(3):
                    H.mm(pA[:], wuq[:, j, h * 128:(h + 1) * 128], cq[:, j, tsl], j == 0, j == 2, [wuq, (cq, tb)], [pA])
                for j in range(3):
                    H.mm(pB[:], wuq[:, j, 1024 + h * 128:1024 + (h + 1) * 128], cq[:, j, tsl], j == 0, j == 2, [wuq, (cq, tb)], [pB])
                a_, b_ = t1[cnt['ti'] % 2], t2[cnt['ti'] % 2]
                cnt['ti'] += 1
                H.tt('dve', a_[:], pA[:], cosT[:, tsl], ALU.mult, [pA, cosT], [a_])
                H.tt('dve', b_[:], pB[:], sinT[:, tsl], ALU.mult, [pB, sinT], [b_])
                H.tt('dve', Q_[:, tsl], a_[:], b_[:], ALU.add, [a_, b_], [(Q_, tb)])
                pK = ps[4 + tb % 2]
                for j in range(2):
                    H.mm(pK[0:64, :], wukv[:, j, h * 128:h * 128 + 64], ckv[:, j, tsl], j == 0, j == 1, [wukv, (ckv, tb)], [pK])
                H.cp('act', K_[0:64, tsl], pK[0:64, :], [pK], [(K_, tb)])
            H.cp('dve', K_[64:128, :], krot[64:128, :], krot_all, [(K_, 'r')])
            vsl = slice((h % 2) * 64, (h % 2) * 64 + 64)
            for g in range(4):
                p_ = ps[2 + (g % 2)]
                for t4 in range(4):
                    tt_ = g * 4 + t4
                    for j in range(2):
                        H.mm(p_[:, t4 * 128:(t4 + 1) * 128], ckv[:, j, tt_ * 128:(tt_ + 1) * 128],
                             wukv[:, j, 1024 + h * 128:1024 + (h + 1) * 128], j == 0, j == 1, [(ckv, tt_ // 4), wukv], [p_])
                H.cp('act', V_[:, g * 4:(g + 1) * 4, vsl], p_[:].rearrange("p (a c) -> p a c", a=4)[:, :, vsl], [p_], [(V_, g)])

        def attn_head(h):
            Q_, K_, V_ = QT[h % 2], KT[h % 2], Vp[h % 2]
            half = slice((h % 2) * 64, (h % 2) * 64 + 64)
            other = slice((1 - h % 2) * 64, (1 - h % 2) * 64 + 64)
            Kr = [(K_, tb) for tb in range(4)] + [(K_, 'r')]
            sbanks = (ps[0], ps[1], ps[4], ps[5])
            LAG = 2
            items = [(qb, kt) for qb in range(4) for kt in range(16)]
            pts = {}
            for i in range(len(items) + LAG):
                if i < len(items):
                    qb, kt = items[i]
                    pS_ = sbanks[i % 4]
                    H.mm(pS_[:], K_[:, kt * 128:(kt + 1) * 128], Q_[:, qb * 512:(qb + 1) * 512], True, True, Kr + [(Q_, qb)], [pS_])
                    pt = PT[cnt['pt'] % 4]
                    cnt['pt'] += 1
                    pts[i] = pt
                    H.actf(pt[:], pS_[:], AF.Exp, [pS_], [pt], scale=ATT_SCALE)
                j = i - LAG
                if j >= 0:
                    qb, kt = items[j]
                    qsl = slice(qb * 512, (qb + 1) * 512)
                    pO = ps[2 + (qb % 2)]
                    pt = pts.pop(j)
                    H.mm(pO[:], V_[:, kt, :], pt[:], kt == 0, kt == 15, [(V_, kt // 4), (V_, 'ones'), pt], [pO])
                    if kt == 15:
                        r_ = rs[qb % 2]
                        H.recip(r_[half, :], pO[other, :], [pO], [r_])
                        H.tt('dve', o1T[half, h // 2, qsl], pO[half, :], r_[half, :], ALU.mult, [pO, r_], [(o1T, h // 2, qb)])

        prep_head(0)
        for h in range(8):
            if h + 1 < 8:
                prep_head(h + 1)
            attn_head(h)
        for pr in range(4):
            H.dma(Sx.o1[b, pr], o1T[:, pr, :], [(o1T, pr, qb) for qb in range(4)], [('o1_d', b, pr)],
                  q='sp' if pr % 2 == 0 else 'pool')


LNX_EPS = 64e-5


def phaseD(G, b):
    H, I, Sx, P = G.H, G.I, G.S, G.P
    ps, pbk = G.ps, G.pb
    nc = G.nc
    with ExitStack() as st:
        wdT = sbt(G, st, 'd_wdT', [128, S], BF16)
        adT = sbt(G, st, 'd_adT', [128, S], BF16)
        gdT = sbt(G, st, 'd_gdT', [128, S], BF16)
        cst = sbt(G, st, 'd_cst', [128, 64], F32)
        H.dma(cst[:], I.rw_cst.ap(), [], [cst])
        MU_P, MU_N, KK_, KA_, RK_, LG_, LB_, A0_ = 0, 15, 30, 34, 38, 42, 46, 50
        c0 = sbt(G, st, 'd_c0', [128, 24], F32)
        H.tt('dve', c0[:, 0:15], cst[:, MU_P:MU_P + 15], cst[:, MU_N:MU_N + 15], ALU.add, [cst], [c0])
        H.ts('dve', c0[:, 0:15], c0[:, 0:15], -1.0, 1.0, ALU.mult, ALU.add, [c0], [c0])
        H.ts('dve', c0[:, 16:20], cst[:, KA_:KA_ + 4], -1.0, 1.0, ALU.mult, ALU.add, [cst], [c0])
        H.ts('dve', c0[:, 20:24], cst[:, KA_:KA_ + 4], -2.0, 2.0, ALU.mult, ALU.add, [cst], [c0])
        bdones = sbt(G, st, 'd_bdones', [128, 128], BF16)
        H.dma(bdones[:], I.bdones.ap(), [], [bdones])
        ident = sbt(G, st, 'd_ident', [128, 128], BF16)
        H.dma(ident[:], I.ident.ap(), [], [ident])
        def mk0(name, shape, dt, n):
            return [[sbt(G, st, '%s_%d_%d' % (name, d, i), shape, dt) for i in range(n)] for d in range(2)]
        AR = mk0('d_AR', [128, 4, 2, 256], BF16, 2)
        Bt = mk0('d_Bt', [128, 4, 2, 128], BF16, 1)
        Kt = mk0('d_Kt', [128, 4, 2, 128], BF16, 1)
        Bh = mk0('d_Bh', [128, 4, 2, 128], BF16, 1)
        Kh = mk0('d_Kh', [128, 4, 2, 128], BF16, 1)
        Vb = mk0('d_Vb', [128, 4, 2, 128], BF16, 1)
        H32 = mk0('d_H32', [128, 4, 128], F32, 1)
        Hbf = mk0('d_Hbf', [128, 4, 128], BF16, 1)
        for d in range(2):
            for lst in (AR, Bt, Kt, Bh, Kh, Vb, H32, Hbf):
                for t_ in lst[d]:
                    H.memset('pool', t_[:], 0.0, [t_])
        with ExitStack() as s1:
            hT = sbt(G, s1, 'd_hT', [128, 8, S], BF16)
            for kt in range(8):
                H.dma(hT[:, kt, :], Sx.hT[b, kt], [('hT_d', b, kt)], [(hT, kt)])
            Wr = sbt(G, s1, 'd_Wr', [128, 8, 1920], BF16)
            H.dma(Wr[:, :, 0:960], Sx.Wbf.ap()[:, :, 672:1632].rearrange("k p c -> p k c"), [('Wbf', k) for k in range(8)], [(Wr, 0)])
            H.dma(Wr[:, :, 960:1920], Sx.Wbf.ap()[:, :, 1632:2592].rearrange("k p c -> p k c"), [('Wbf', k) for k in range(8)], [(Wr, 1)])
            zt = [sbt(G, s1, 'd_zt%d' % i, [128, S + 2], F32) for i in range(2)]
            acc = [sbt(G, s1, 'd_acc%d' % i, [128, S], F32) for i in range(2)]
            kf = sbt(G, s1, 'd_kf', [128, S], F32)
            sqk = sbt(G, s1, 'd_sqk', [128, S], BF16)
            rk = [sbt(G, s1, 'd_rk%d' % i, [128, 512], F32) for i in range(2)]
            ob = [sbt(G, s1, 'd_ob%d' % i, [128, S], BF16) for i in range(3)]
            obi = [0]

            def emit_out(qi, j, producer):
                o_ = ob[obi[0] % 3]
                obi[0] += 1
                producer(o_)
                H.dma(Sx.rw[b, qi, j], o_[:], [o_] + [(o_, t4) for t4 in range(4)], [('rw', b, qi, j)])
            for z_ in zt:
                H.memset('pool', z_[:, 0:1], 0.0, [(z_, 'l')])
                H.memset('pool', z_[:, S + 1:S + 2], 0.0, [(z_, 'r')])
            pi = 0
            for ct in range(15):
                z_, a_ = zt[ct % 2], acc[ct % 2]
                for tb in range(4):
                    p_ = ps[pi % 4]
                    pi += 1
                    for kt in range(8):
                        H.mm(p_[:], Wr[:, kt, ct * 128:(ct + 1) * 128], hT[:, kt, tb * 512:(tb + 1) * 512], kt == 0, kt == 7,
                             [(Wr, 0), (Wr, 1), (hT, kt)], [p_])
                    H.cp('act', z_[:, 1 + tb * 512:1 + (tb + 1) * 512], p_[:], [p_], [(z_, tb)])
                    H.actf(a_[:, tb * 512:(tb + 1) * 512], p_[:], AF.Copy, [p_, c0, a_], [a_], scale=c0[:, ct:ct + 1])
                zall = [(z_, tb) for tb in range(4)] + [(z_, 'l'), (z_, 'r')]
                H.stt('pool', a_[:], z_[:, 0:S], cst[:, MU_P + ct:MU_P + ct + 1], a_[:], ALU.mult, ALU.add, zall + [cst, a_], [a_])
                if ct < 4:
                    emit_out(0, ct, lambda o_: H.stt('dve', o_[:], z_[:, 2:S + 2], cst[:, MU_N + ct:MU_N + ct + 1], a_[:], ALU.mult, ALU.add, zall + [cst, a_], [o_]))
                elif ct < 8:
                    j = ct - 4
                    H.stt('dve', kf[:], z_[:, 2:S + 2], cst[:, MU_N + ct:MU_N + ct + 1], a_[:], ALU.mult, ALU.add, zall + [cst, a_], [kf])
                    emit_out(3, j, lambda o_: H.cp('act', o_[:], kf[:], [kf], [o_]))
                    H.ts('dve', kf[:], kf[:], cst[:, KK_ + j:KK_ + j + 1], None, ALU.mult, None, [kf, cst], [kf])
                    def kkprod(o_):
                        H.actf(sqk[:], kf[:], AF.Square, [kf], [sqk])
                        for tb in range(4):
                            tsl = slice(tb * 512, (tb + 1) * 512)
                            p2 = ps[4 + tb % 2]
                            rk_ = rk[tb % 2]
                            H.mm(p2[:], bdones[:], sqk[:, tsl], True, True, [bdones, sqk], [p2])
                            H.rsqrt_act(rk_[:], p2[:], 1.0, 1e-24, [p2], [rk_])
                            H.tt('dve', o_[:, tsl], kf[:, tsl], rk_[:], ALU.mult, [kf, rk_], [(o_, tb)])
                    emit_out(2, j, kkprod)
                elif ct < 12:
                    emit_out(1, ct - 8, lambda o_: H.stt('dve', o_[:], z_[:, 2:S + 2], cst[:, MU_N + ct:MU_N + ct + 1], a_[:], ALU.mult, ALU.add, zall + [cst, a_], [o_]))
                else:
                    H.stt('dve', a_[:], z_[:, 2:S + 2], cst[:, MU_N + ct:MU_N + ct + 1], a_[:], ALU.mult, ALU.add, zall + [cst, a_], [a_])
                    if ct == 12:
                        H.actf(wdT[:], a_[:], AF.Tanh, [a_], [wdT])
                    elif ct == 13:
                        H.cp('act', adT[:], a_[:], [a_], [adT])
                    else:
                        H.actf(gdT[:], a_[:], AF.Sigmoid, [a_], [gdT])
        P.barrier()
        yT = sbt(G, st, 'd_yT', [128, 4, S], F32)
        H.memset('pool', yT[:], 0.0, [yT])
        with ExitStack() as s2:
            stg = sbt(G, s2, 'd_stg', [128, 512], F32)
            wup = sbt(G, s2, 'd_wup', [128, 512], BF16)
            aup = sbt(G, s2, 'd_aup', [128, 512], BF16)
            H.dma(stg[:], I.w_up.ap(), [], [stg])
            H.cp('dve', wup[:], stg[:], [stg], [wup])
            H.dma(stg[:], I.a_up.ap(), [], [stg])
            H.cp('dve', aup[:], stg[:], [stg], [aup])
            w0b = sbt(G, s2, 'd_w0b', [128, 2, 512], F32)
            H.dma(w0b[:], I.w0b.ap().rearrange("d p c -> p d c"), [], [w0b])
            tri3 = sbt(G, s2, 'd_tri3', [128, 2, 384], F32)
            H.dma(tri3[:], I.tri3.ap().rearrange("d p c -> p d c"), [], [tri3])
            maskT = sbt(G, s2, 'd_maskT', [128, 2, 512], BF16)
            H.dma(maskT[:], I.maskT.ap().rearrange("d p c -> p d c"), [], [maskT])
            maskL = sbt(G, s2, 'd_maskL', [128, 2, 512], BF16)
            H.dma(maskL[:], I.maskL.ap().rearrange("d p c -> p d c"), [], [maskL])
            ident4 = sbt(G, s2, 'd_ident4', [128, 512], BF16)
            H.dma(ident4[:], I.ident4.ap(), [], [ident4])

            def mk(name, shape, dt, n):
                return [[sbt(G, s2, '%s_%d_%d' % (name, d, i), shape, dt) for i in range(n)] for d in range(2)]
            sg = mk('d_sg', [128, 512], F32, 1)
            E3 = mk('d_E3', [128, 4, 384], F32, 1)
            ld = mk('d_ld', [128, 4, 4, 128], BF16, 1)
            fac = mk('d_fac', [128, 4, 128], F32, 1)
            Einv = mk('d_Einv', [128, 4, 128], F32, 1)
            a_t = mk('d_at', [128, 4, 128], F32, 1)
            bq = mk('d_bq', [128, 4, 128], BF16, 1)
            kd = mk('d_kd', [128, 4, 128], BF16, 1)
            gC = mk('d_gC', [128, 4, 2], F32, 2)
            TTb = mk('d_TTb', [128, 4, 128], BF16, 2)
            E1 = mk('d_E1', [128, 4, 256], BF16, 2)
            E2 = mk('d_E2', [128, 4, 256], BF16, 2)
            E3l = mk('d_E3l', [128, 4, 128], BF16, 2)
            QL = mk('d_QL', [128, 4, 256], BF16, 2)
            Ak = mk('d_Ak', [128, 4, 128], BF16, 2)
            TM = mk('d_TM', [128, 4, 384], BF16, 2)
            Xs = mk('d_Xs', [128, 4, 128], BF16, 1)
            Us = mk('d_Us', [128, 4, 128], BF16, 1)
            Ys = mk('d_Ys', [128, 4, 128], F32, 1)
            ytmp = mk('d_ytmp', [128, 4, 64], F32, 1)
            unit = [0]

            def nextbank():
                u = unit[0]
                unit[0] += 1
                return (ps[0], ps[1], ps[4], ps[5])[u % 4]
            ecnt = [0]

            def eng2():
                ecnt[0] += 1
                return 'dve'

            def prep_tile(d, tt):
                par = tt % 2
                tsl = slice(tt * 128, (tt + 1) * 128)
                dsl = slice(d * 64, (d + 1) * 64)
                sg_, tw_, E3_, Ei_, at_, bq_, kd_ = sg[d][0], sg[d][0], E3[d][0], Einv[d][0], a_t[d][0], bq[d][0], kd[d][0]
                ld_ = ld[d][0]
                for qi in range(4):
                    H.dma(ld_[:, qi, :, :], Sx.rw.ap()[b, qi, :, :, tsl].rearrange("h p t -> p h t"),
                          [('rw', b, qi, j) for j in range(4)], [(ld_, qi)])
                r_t, v_t, kk_t, k_t = ld_[:, 0], ld_[:, 1], ld_[:, 2], ld_[:, 3]
                fac_ = fac[d][0]
                pw = nextbank()
                H.mm(pw[:], wdT[dsl, tsl], wup[dsl, :], True, True, [wdT, wup], [pw])
                H.tt('dve', tw_[:], pw[:], w0b[:, d, :], ALU.add, [pw, w0b], [tw_])
                H.actf(sg_[:], tw_[:], AF.Sigmoid, [tw_], [sg_])
                for hp in range(4):
                    pc = nextbank()
                    H.mm(pc[:, 0:384], sg_[:, hp * 128:(hp + 1) * 128], tri3[:, d, :], True, True, [sg_, tri3], [pc])
                    H.actf(E3_[:, hp, :], pc[:, 0:384], AF.Exp, [pc], [(E3_, hp)])
                    H.actf(Ei_[:, hp, :], pc[:, 0:128], AF.Exp, [pc], [(Ei_, hp)], scale=-1.0)
                E3all = [(E3_, hp) for hp in range(4)]
                Eiall = [(Ei_, hp) for hp in range(4)]
                pa = nextbank()
                for hp in range(4):
                    H.mm(pa[:, hp * 128:(hp + 1) * 128], aup[dsl, hp * 128:(hp + 1) * 128], adT[dsl, tsl], True, True, [aup, adT], [pa])
                for hp in range(4):
                    H.actf(at_[:, hp, :], pa[:, hp * 128:(hp + 1) * 128], AF.Sigmoid, [pa, cst], [(at_, hp)],
                           bias=cst[:, A0_ + d * 4 + hp:A0_ + d * 4 + hp + 1])
                atall = [(at_, hp) for hp in range(4)]
                if getattr(P, '_cap', None) is not None:
                    P._cap.append('MARK')
                H.tt('pool', bq_[:], kk_t, at_[:], ALU.mult, [(ld_, 2)] + atall, [bq_])
                for hp in range(4):
                    H.ts(eng2(), fac_[:, hp, :], at_[:, hp, :], cst[:, KA_ + hp:KA_ + hp + 1], c0[:, 16 + hp:17 + hp], ALU.mult, ALU.add,
                         [(at_, hp), cst, c0], [(fac_, hp)])
                H.tt('dve', kd_[:], k_t, fac_[:], ALU.mult, [(ld_, 3)] + [(fac_, hp) for hp in range(4)], [kd_])
                AR_, Bt_, Kt_, Bh_, Kh_, Vb_ = AR[d][par], Bt[d][0], Kt[d][0], Bh[d][0], Kh[d][0], Vb[d][0]
                gcols = (63, 127) if d == 0 else (0, 64)
                for c2_ in range(2):
                    H.cp('pool', gC[d][par][:, :, c2_:c2_ + 1], E3_[:, :, gcols[c2_]:gcols[c2_] + 1], E3all, [gC[d][par]])

                def v4(ap):
                    return ap.rearrange("p h (c t) -> p h c t", c=2)
                for hf in range(2):
                    psl = slice(hf * 64, hf * 64 + 64)
                    csl = slice(hf * 64, hf * 64 + 64)
                    gi, gp, gs = E3_[psl, :, 0:128], E3_[psl, :, 128:256], E3_[psl, :, 256:384]
                    H.stt(eng2(), AR_[psl, :, :, csl], v4(ld_[psl, 2]), -1.0, v4(gp), ALU.mult, ALU.mult,
                          [(ld_, 2)] + E3all, [(AR_, hf, 0)])
                    H.tt(eng2(), AR_[psl, :, :, 128 + hf * 64:128 + hf * 64 + 64], v4(ld_[psl, 0]), v4(gi), ALU.mult,
                         [(ld_, 0)] + E3all, [(AR_, hf, 1)])
                    H.tt(eng2(), Bt_[psl, :, :, csl], v4(bq_[psl, :, :]), v4(Ei_[psl, :, :]), ALU.mult, [bq_] + Eiall, [(Bt_, hf)])
                    H.tt(eng2(), Kt_[psl, :, :, csl], v4(kd_[psl, :, :]), v4(Ei_[psl, :, :]), ALU.mult, [kd_] + Eiall, [(Kt_, hf)])
                    H.cp(eng2(), Vb_[psl, :, :, csl], v4(ld_[psl, 1]), [(ld_, 1)], [(Vb_, hf)])

            def res2(t_):
                return [(t_, 0), (t_, 1)]

            def rARf(AR_):
                return [(AR_, 0, 0), (AR_, 1, 0), (AR_, 0, 1), (AR_, 1, 1)]

            def P0(d, tt, c2, n):
                par, np_ = tt % 2, n % 2
                AR_, Bt_, Kt_ = AR[d][par], Bt[d][0], Kt[d][0]
                rAR = rARf(AR_)
                for (lt, dst) in ((Bt_, E1[d][np_]), (Kt_, E2[d][np_])):
                    for pr in range(2):
                        pk = nextbank()
                        for q in range(2):
                            hp = pr * 2 + q
                            H.mm(pk[:, q * 256:(q + 1) * 256], lt[:, hp, c2, :], AR_[:, hp, c2, :], True, True, res2(lt) + rAR, [pk])
                        H.tt('dve', dst[:, pr * 2:pr * 2 + 2, :], pk[:].rearrange("p (a c) -> p a c", a=2),
                             maskT[:, d, :].rearrange("p (a c) -> p a c", a=2), ALU.mult, [pk, maskT], [(dst, pr)])
                pk = nextbank()
                for hp in range(4):
                    H.mm(pk[:, hp * 128:(hp + 1) * 128], AR_[:, hp, c2, 0:128], Bt_[:, hp, c2, :], True, True, res2(Bt_) + rAR, [pk])
                H.tt('dve', E3l[d][np_][:], pk[:].rearrange("p (a c) -> p a c", a=4),
                     maskL[:, d, :].rearrange("p (a c) -> p a c", a=4), ALU.mult, [pk, maskL], [E3l[d][np_]])
                H.tt('pool', Ak[d][0][:], E1[d][np_][:, :, 0:128], ident4[:].rearrange("p (a c) -> p a c", a=4), ALU.add,
                     [(E1[d][np_], 0), (E1[d][np_], 1), ident4], [Ak[d][0]])
                Bh_, Kh_, Vb_ = Bt[d][0], Kt[d][0], Vb[d][0]
                tm = TM[d][np_]
                pb0, pb1 = pbk[0], pbk[1]
                for hp in range(4):
                    H.tr(pb0[:, hp * 256:hp * 256 + 128], Bh_[:, hp, c2, :], ident[:], res2(Bh_) + [ident], [pb0])
                    H.tr(pb0[:, hp * 256 + 128:hp * 256 + 256], Kh_[:, hp, c2, :], ident[:], res2(Kh_) + [ident], [pb0])
                H.cp('act', tm[:, :, 0:256], pb0[:].rearrange("p (a c) -> p a c", a=4), [pb0], [(tm, 0)])
                for hp in range(4):
                    H.tr(pb1[:, hp * 128:(hp + 1) * 128], Vb_[:, hp, c2, :], ident[:], res2(Vb_) + [ident], [pb1])
                H.cp('act', tm[:, :, 256:384], pb1[:, 0:512].rearrange("p (a c) -> p a c", a=4), [pb1], [(tm, 1)])

            def PQ(d, n, k):
                np_ = n % 2
                e1, e3l = E1[d][np_], E3l[d][np_]
                qprev = QL[d][(k - 1) % 2]
                qcur = QL[d][k % 2]

                def Qp(hp):
                    return e1[:, hp, 0:128] if k == 1 else qprev[:, hp, 0:128]

                def Lp(hp):
                    return e3l[:, hp, :] if k == 1 else qprev[:, hp, 128:256]
                for pr in range(2):
                    rprev = [(e1, pr), e3l] if k == 1 else [(qprev, pr)]
                    pk = nextbank()
                    for q in range(2):
                        hp = pr * 2 + q
                        if k < 5:
                            H.mm(pk[:, q * 256:q * 256 + 128], Lp(hp), Qp(hp), True, True, rprev, [pk])
                        H.mm(pk[:, q * 256 + 128:q * 256 + 256], Qp(hp), Lp(hp), True, True, rprev, [pk])
                    if k < 5:
                        H.cp('act', qcur[:, pr * 2:pr * 2 + 2, :], pk[:].rearrange("p (a c) -> p a c", a=2), [pk], [(qcur, pr)])
                    else:
                        H.cp('act', qcur[:, pr * 2:pr * 2 + 2, 128:256], pk[:].rearrange("p (a c) -> p a c", a=2)[:, :, 128:256], [pk], [(qcur, pr)])

            def PA(d, n, k):
                qcur = QL[d][k % 2]
                aprev = Ak[d][(k - 1) % 2]
                acur = Ak[d][k % 2] if k < 5 else TTb[d][n % 2]
                pk = nextbank()
                for hp in range(4):
                    H.mm(pk[:, hp * 128:(hp + 1) * 128], qcur[:, hp, 128:256], aprev[:, hp, :], True, True, [(qcur, 0), (qcur, 1), aprev], [pk])
                H.tt('dve', acur[:], pk[:].rearrange("p (a c) -> p a c", a=4), aprev[:], ALU.add, [pk, aprev], [acur])

            def S0(d, tt, c2, n):
                par, np_ = tt % 2, n % 2
                AR_ = AR[d][par]
                e2, tm = E2[d][np_], TM[d][np_]
                SX = ps[2 + d]
                hbf, xs = Hbf[d][0], Xs[d][0]
                for hp in range(4):
                    o = SX[:, hp * 128:(hp + 1) * 128]
                    H.mm(o, AR_[:, hp, c2, 0:128], hbf[:, hp, :], True, False, rARf(AR_) + [hbf], [SX])
                    H.mm(o, e2[:, hp, 0:128], tm[:, hp, 256:384], False, True, [(e2, 0), (e2, 1), (tm, 1)], [SX])
                H.cp('act', xs[:], SX[:].rearrange("p (a c) -> p a c", a=4), [SX], [xs])

            def S1(d, tt, c2, n):
                SX = ps[2 + d]
                TT_ = TTb[d][n % 2]
                xs, us = Xs[d][0], Us[d][0]
                for hp in range(4):
                    H.mm(SX[:, hp * 128:(hp + 1) * 128], TT_[:, hp, :], xs[:, hp, :], True, True, [TT_, xs], [SX])
                H.cp('act', us[:], SX[:].rearrange("p (a c) -> p a c", a=4), [SX], [us])

            def S2(d, tt, c2, n):
                par, np_ = tt % 2, n % 2
                AR_ = AR[d][par]
                rAR = rARf(AR_)
                e1, e2, tm = E1[d][np_], E2[d][np_], TM[d][np_]
                SX, SH = ps[2 + d], ps[2 + d]
                hbf, h32, us, ys, yt_ = Hbf[d][0], H32[d][0], Us[d][0], Ys[d][0], ytmp[d][0]
                for hp in range(4):
                    o = SX[:, hp * 128:(hp + 1) * 128]
                    H.mm(o, hbf[:, hp, :], AR_[:, hp, c2, 128:256], True, False, rAR + [hbf], [SX])
                    H.mm(o, us[:, hp, :], e1[:, hp, 128:256], False, False, [us, (e1, 0), (e1, 1)], [SX])
                    H.mm(o, tm[:, hp, 256:384], e2[:, hp, 128:256], False, True, [(tm, 1), (e2, 0), (e2, 1)], [SX])
                H.cp('act', ys[:], SX[:].rearrange("p (a c) -> p a c", a=4), [SX], [ys])
                for hp in range(4):
                    o = SH[:, hp * 128:(hp + 1) * 128]
                    H.mm(o, tm[:, hp, 0:128], us[:, hp, :], True, False, [(tm, 0), us], [SH])
                    H.mm(o, tm[:, hp, 128:256], tm[:, hp, 256:384], False, True, [(tm, 0), (tm, 1)], [SH])
                csl = slice(tt * 128 + c2 * 64, tt * 128 + c2 * 64 + 64)
                H.tt('pool', yt_[:], ys[:, :, 0:64], ys[:, :, 64:128], ALU.add, [ys], [yt_])
                H.tt('pool', yT[:, :, csl], yT[:, :, csl], yt_[:], ALU.add, [yT, yt_], [yT])
                gC_ = gC[d][par]
                H.tt('dve', h32[:], h32[:], SH[:].rearrange("p (a c) -> p a c", a=4), ALU.add, [h32, SH], [h32])
                for hp in range(4):
                    H.ts('dve', h32[:, hp, :], h32[:, hp, :], gC_[:, hp, c2:c2 + 1], None, ALU.mult, None, [h32, gC_], [h32])
                H.cp('pool', hbf[:], h32[:], [h32], [hbf])

            def step_info(n):
                i, c = n // 2, n % 2
                return [(i, c), (15 - i, 1 - c)]

            def capture(fn):
                ops = []
                P._cap = ops
                P.add = lambda E, f, r=(), w=(), dma=False: ops.append((E, f, r, w, dma))
                try:
                    fn()
                finally:
                    del P.add
                    P._cap = None
                return ops

            pending = []

            def drip(frac_left):
                if not pending:
                    return
                k = -(-len(pending) // max(frac_left, 1))
                for _ in range(min(k, len(pending))):
                    P.add(*pending.pop(0))

            for d in range(2):
                prep_tile(d, (0, 15)[d])
            NS = 32 if 'noD2' not in G.dbg else 0
            for n in range(-1, NS):
                nx = n + 1
                if nx < NS:
                    inf = step_info(nx)
                    for d in range(2):
                        P0(d, inf[d][0], inf[d][1], nx)
                if n >= 0:
                    cur = step_info(n)
                    for d in range(2):
                        S0(d, cur[d][0], cur[d][1], n)
                if n >= 0 and n % 2 == 0 and n + 2 < NS:
                    i2 = (n + 2) // 2
                    a_ops = capture(lambda: prep_tile(0, i2))
                    b_ops = capture(lambda: prep_tile(1, 15 - i2))
                    for lst in (a_ops, b_ops):
                        mk_ = lst.index('MARK')
                        for op_ in lst[:mk_]:
                            P.add(*op_)
                        del lst[:mk_ + 1]
                    m_ = max(len(a_ops), len(b_ops))
                    for j in range(m_):
                        if j < len(a_ops):
                            pending.append(a_ops[j])
                        if j < len(b_ops):
                            pending.append(b_ops[j])
                drip(6)
                if nx < NS:
                    for d in range(2):
                        PQ(d, nx, 1)
                if n >= 0:
                    for d in range(2):
                        S1(d, cur[d][0], cur[d][1], n)
                drip(5)
                if nx < NS:
                    for d in range(2):
                        PQ(d, nx, 2)
                    for d in range(2):
                        PA(d, nx, 1)
                if n >= 0:
                    for d in range(2):
                        S2(d, cur[d][0], cur[d][1], n)
                drip(4)
                if nx < NS:
                    for k in (3, 4, 5):
                        for d in range(2):
                            PQ(d, nx, k)
                        for d in range(2):
                            PA(d, nx, k - 1)
                        drip(6 - k)
                    for d in range(2):
                        PA(d, nx, 5)
                while pending:
                    P.add(*pending.pop(0))
        P.barrier()
        with ExitStack() as s3:
            bdf = sbt(G, s3, 'd_bdf', [128, 128], F32)
            H.dma(bdf[:], I.bdones_f.ap(), [], [bdf])
            stg = sbt(G, s3, 'd_stg3', [128, 512], F32)
            gup = sbt(G, s3, 'd_gup', [128, 512], BF16)
            H.dma(stg[:], I.g_up.ap(), [], [stg])
            H.cp('dve', gup[:], stg[:], [stg], [gup])
            o2T = sbt(G, s3, 'd_o2T', [128, 4, S], BF16)
            NB3 = 3

            def m3(name, dt):
                return [sbt(G, s3, 'd_%s%d' % (name, i), [128, 512], dt) for i in range(NB3)]
            yc, sq, rs, bo, as0, as1 = m3('yc', F32), m3('sq3', F32), m3('rs3', F32), m3('bo', F32), m3('as0', F32), m3('as1', F32)
            sb_, kl, kds, rl, vl = m3('sb', BF16), m3('kl', BF16), m3('kds', BF16), m3('rl', BF16), m3('vl', BF16)
            aup3 = sbt(G, s3, 'd_aup3', [128, 512], BF16)
            H.dma(stg[:], I.a_up.ap(), [gup], [stg])
            H.cp('dve', aup3[:], stg[:], [stg], [aup3])

            def d3_block(bi):
                hp, tb = bi // 4, bi % 4
                c = bi % NB3
                tsl = slice(tb * 512, (tb + 1) * 512)
                rl_, vl_, kl_ = rl[c], vl[c], kl[c]
                y_, q_, r_, s_, b_, a0_, a1_, kd_ = yc[c], sq[c], rs[c], sb_[c], bo[c], as0[c], as1[c], kds[c]
                bA, bB = ps[2 * c], ps[2 * c + 1]
                H.dma(rl_[:], Sx.rw[b, 0, hp, :, tsl], [('rw', b, 0, hp)], [rl_])
                H.dma(vl_[:], Sx.rw[b, 1, hp, :, tsl], [('rw', b, 1, hp)], [vl_])
                H.dma(kl_[:], Sx.rw[b, 3, hp, :, tsl], [('rw', b, 3, hp)], [kl_])
                H.mm(bA[:], bdf[:], yT[:, hp, tsl], True, True, [bdf, yT], [bA])
                H.mm(bB[:], aup3[0:64, hp * 128:(hp + 1) * 128], adT[0:64, tsl], True, True, [aup3, adT], [bB])
                H.stt('dve', y_[:], bA[:], -1.0 / 64, yT[:, hp, tsl], ALU.mult, ALU.add, [bA, yT], [y_])
                H.actf(a0_[:], bB[:], AF.Sigmoid, [bB, cst], [a0_], bias=cst[:, A0_ + hp:A0_ + hp + 1])
                H.actf(q_[:], y_[:], AF.Square, [y_], [q_])
                H.mm(bB[:], aup3[64:128, hp * 128:(hp + 1) * 128], adT[64:128, tsl], True, True, [aup3, adT], [bB])
                H.mm(bA[:], bdf[:], q_[:], True, True, [bdf, q_], [bA])
                H.actf(a1_[:], bB[:], AF.Sigmoid, [bB, cst], [a1_], bias=cst[:, A0_ + 4 + hp:A0_ + 4 + hp + 1])
                H.rsqrt_act(r_[:], bA[:], 1.0 / 64, LNX_EPS, [bA], [r_])
                H.tt('pool', a0_[:], a0_[:], a1_[:], ALU.add, [a0_, a1_], [a0_])
                H.tt('pool', y_[:], y_[:], r_[:], ALU.mult, [y_, r_], [y_])
                H.ts('dve', a0_[:], a0_[:], cst[:, KA_ + hp:KA_ + hp + 1], c0[:, 20 + hp:21 + hp], ALU.mult, ALU.add, [a0_, cst, c0], [a0_])
                H.ts('dve', y_[:], y_[:], cst[:, LG_ + hp:LG_ + hp + 1], cst[:, LB_ + hp:LB_ + hp + 1], ALU.mult, ALU.add, [y_, cst], [y_])
                H.tt('pool', kd_[:], kl_[:], a0_[:], ALU.mult, [kl_, a0_], [kd_])
                H.stt('dve', s_[:], rl_[:], cst[:, RK_ + hp:RK_ + hp + 1], kd_[:], ALU.mult, ALU.mult, [rl_, cst, kd_], [s_])
                H.mm(bB[:], bdones[:], s_[:], True, True, [bdones, s_], [bB])
                H.mm(bA[:], gup[:, hp * 128:(hp + 1) * 128], gdT[:, tsl], True, True, [gup, gdT], [bA])
                H.tt('dve', b_[:], bB[:], vl_[:], ALU.mult, [bB, vl_], [b_])
                H.tt('pool', y_[:], y_[:], b_[:], ALU.add, [y_, b_], [y_])
                H.tt('dve', o2T[:, hp, tsl], bA[:], y_[:], ALU.mult, [bA, y_], [(o2T, hp)])

            def capture3(fn):
                ops = []
                P.add = lambda E, f, r=(), w=(), dma=False: ops.append((E, f, r, w, dma))
                try:
                    fn()
                finally:
                    del P.add
                return ops

            NBLK = 16 if 'noD3' not in G.dbg else 0
            for g0 in range(0, NBLK, NB3):
                chains = [capture3(lambda bi=bi: d3_block(bi)) for bi in range(g0, min(g0 + NB3, NBLK))]
                idx = [0] * len(chains)
                live = True
                while live:
                    live = False
                    for ci, ops_ in enumerate(chains):
                        if idx[ci] < len(ops_):
                            P.add(*ops_[idx[ci]])
                            idx[ci] += 1
                            live = True
            for pr in range(4):
                H.dma(Sx.o2[b, pr], o2T[:, pr, :], [(o2T, pr)], [('o2_d', b, pr)], q='sp' if pr % 2 == 0 else 'pool')


CAP = 384
NSLOT = CAP // 128


def phaseE(G, b):
    H, I, Sx, P = G.H, G.I, G.S, G.P
    ps = G.ps
    with ExitStack() as st:
        hT = sbt(G, st, 'e_hT', [128, 8, S], BF16)
        for kt in range(8):
            H.dma(hT[:, kt, :], Sx.hT[b, kt], [('hT_d', b, kt)], [(hT, kt)])
        o1T = sbt(G, st, 'e_o1T', [128, 4, S], BF16)
        o2T = sbt(G, st, 'e_o2T', [128, 4, S], BF16)
        for pr in range(4):
            H.dma(o1T[:, pr, :], Sx.o1[b, pr], [('o1_d', b, pr)], [(o1T, pr)])
            H.dma(o2T[:, pr, :], Sx.o2[b, pr], [('o2_d', b, pr)], [(o2T, pr)], q='pool')
        Wg = sbt(G, st, 'e_Wg', [128, 8, 2048], BF16)
        H.dma(Wg[:, :, 0:1024], Sx.Wbf.ap()[:, :, 2592:3616].rearrange("k p c -> p k c"), [('Wbf', k) for k in range(8)], [(Wg, 0)])
        H.dma(Wg[:, :, 1024:2048], Sx.Wbf.ap()[:, :, 3616:4640].rearrange("k p c -> p k c"), [('Wbf', k) for k in range(8)], [(Wg, 1)], q='pool')
        wbm = sbt(G, st, 'e_wbm', [128, 4, 1024], BF16)
        wbr = sbt(G, st, 'e_wbr', [128, 4, 1024], BF16)
        wout = sbt(G, st, 'e_wout', [128, 8, 1024], BF16)
        H.dma(wbm[:], Sx.wbm.ap().rearrange("k p c -> p k c"), [('wbm',)], [wbm])
        H.dma(wbr[:], Sx.wbr.ap().rearrange("k p c -> p k c"), [('wbr',)], [wbr])
        H.dma(wout[:], Sx.wout.ap().rearrange("k p c -> p k c"), [('wout0',), ('wout1',)], [wout])
        gb = sbt(G, st, 'e_gb', [128, 16], F32)
        H.dma(gb[:], I.gate_b.ap(), [], [gb])
        if b == 0:
            zz = sbt(G, st, 'e_zz', [128, 12 * D], BF16)
            H.memset('pool', zz[:], 0.0, [zz])
            for c8 in range(8):
                H.dma(Sx.xg[c8 * 1536:(c8 + 1) * 1536, :].rearrange("(p a) c -> p a c", a=12), zz[:].rearrange("p (a c) -> p a c", a=12),
                      [zz], [('xgz', c8)], q='pool')
        mT = [sbt(G, st, 'e_mT%d' % i, [128, 8, 512], BF16) for i in range(2)]
        g1 = [sbt(G, st, 'e_g1%d' % i, [128, 512], F32) for i in range(2)]
        g2 = [sbt(G, st, 'e_g2%d' % i, [128, 512], F32) for i in range(2)]
        xt = [sbt(G, st, 'e_xt%d' % i, [128, D], F32) for i in range(2)]
        it = 0
        xi = 0
        for tb in range(4):
            tsl = slice(tb * 512, (tb + 1) * 512)
            m_ = mT[tb % 2]
            for dt in range(8):
                dsl = slice(dt * 128, (dt + 1) * 128)
                pg1, pg2, pb1, pb2 = ps[0], ps[1], ps[2], ps[3]
                for kt in range(8):
                    H.mm(pg1[:], Wg[:, kt, dsl], hT[:, kt, tsl], kt == 0, kt == 7, [(Wg, 0), (hT, kt)], [pg1])
                for kt in range(8):
                    H.mm(pg2[:], Wg[:, kt, 1024 + dt * 128:1024 + (dt + 1) * 128], hT[:, kt, tsl], kt == 0, kt == 7, [(Wg, 1), (hT, kt)], [pg2])
                for j in range(4):
                    H.mm(pb1[:], wbm[:, j, dsl], o1T[:, j, tsl], j == 0, j == 3, [wbm, (o1T, j)], [pb1])
                for j in range(4):
                    H.mm(pb2[:], wbr[:, j, dsl], o2T[:, j, tsl], j == 0, j == 3, [wbr, (o2T, j)], [pb2])
                a_, b_ = g1[it % 2], g2[it % 2]
                it += 1
                H.actf(a_[:], pg1[:], AF.Sigmoid, [pg1, gb], [a_], bias=gb[:, dt:dt + 1])
                H.actf(b_[:], pg2[:], AF.Sigmoid, [pg2, gb], [b_], bias=gb[:, 8 + dt:9 + dt])
                H.tt('dve', a_[:], a_[:], pb1[:], ALU.mult, [a_, pb1], [a_])
                H.tt('dve', b_[:], b_[:], pb2[:], ALU.mult, [b_, pb2], [b_])
                H.tt('dve', m_[:, dt, :], a_[:], b_[:], ALU.add, [a_, b_], [(m_, dt)])
            for t4 in range(4):
                x_ = xt[xi % 2]
                xi += 1
                r0 = b * S + tb * 512 + t4 * 128
                H.dma(x_[:], I.x[r0:r0 + 128, :], [], [x_])
                for half in range(2):
                    po = ps[4 + half]
                    for dt in range(8):
                        H.mm(po[:], m_[:, dt, t4 * 128:(t4 + 1) * 128], wout[:, dt, half * 512:(half + 1) * 512], dt == 0, dt == 7,
                             [(m_, dt), wout], [po])
                    H.tt('dve', x_[:, half * 512:(half + 1) * 512], x_[:, half * 512:(half + 1) * 512], po[:], ALU.add, [x_, po], [x_])
                H.dma(Sx.x1[r0:r0 + 128, :], x_[:], [x_], [('x1', r0)], q='pool')


def phaseF(G):
    H, I, Sx, P = G.H, G.I, G.S, G.P
    ps, pbk = G.ps, G.pb
    nc = G.nc
    NT = G.nseq * 16
    IOA = bass.IndirectOffsetOnAxis
    with ExitStack() as st:
        slots = [sbt(G, st, 'f_slot%d' % k, [128, NT], I32) for k in range(2)]
        cw = [sbt(G, st, 'f_cw%d' % k, [128, NT], F32) for k in range(2)]
        gfb = sbt(G, st, 'f_gfb', [128, 8, 256], F32)
        H.dma(gfb[:], I.gffn_b.ap(), [], [gfb])
        ident = sbt(G, st, 'f_ident', [128, 128], BF16)
        H.dma(ident[:], I.ident.ap(), [], [ident])
        with ExitStack() as s1:
            identf = sbt(G, s1, 'f_identf', [128, 128], F32)
            H.dma(identf[:], I.ident_f.ap(), [], [identf])
            ones = sbt(G, s1, 'f_ones', [128, 128], BF16)
            H.dma(ones[:], I.ones.ap(), [], [ones])
            tris = sbt(G, s1, 'f_tris', [128, 128], BF16)
            H.dma(tris[:], I.tris.ap(), [], [tris])
            RW = sbt(G, s1, 'f_RW', [128, 8, 36], F32)
            H.dma(RW[:], I.rw_router.ap(), [], [RW])
            H.tt('dve', RW[:], RW[:], gfb[:, :, 0:36], ALU.mult, [RW, gfb], [RW])
            rbias = sbt(G, s1, 'f_rbias', [128, 36], F32)
            H.dma(rbias[:], I.rbias_b.ap(), [], [rbias])
            ecap = sbt(G, s1, 'f_ecap', [128, 32], F32)
            H.dma(ecap[:], I.ecap_b.ap(), [], [ecap])
            gfrow = sbt(G, s1, 'f_gfrow', [128, D], F32)
            H.dma(gfrow[:], I.gffn_row_b.ap(), [], [gfrow])
            NB = 4
            ecapa = sbt(G, s1, 'f_ecapa', [128, NT, 32], F32)
            H.dma(ecapa[:], I.ecap_all.ap()[:, 0:NT, :], [], [ecapa])
            hb_all = sbt(G, s1, 'f_hball', [128, NT, D], BF16)
            oh_all = sbt(G, s1, 'f_ohall', [128, 2, NT, 32], F32)
            Abf_all = sbt(G, s1, 'f_Abfall', [128, NT, 32], BF16)
            pos_all = sbt(G, s1, 'f_posall', [128, NT, 32], F32)
            prod_all = sbt(G, s1, 'f_prodall', [128, NT, 32], F32)
            slf_all = sbt(G, s1, 'f_slfall', [128, 2, NT], F32)

            def sm(name, shape, dt=F32):
                return [sbt(G, s1, 'f_%s%d' % (name, i), shape, dt) for i in range(NB)]
            xt, sq, h2T = sm('xt', [128, D]), sm('sq', [128, D]), sm('h2T', [128, 8, 128])
            ss, lg, gmax, goh, ex, gsum, sel = sm('ss', [128, 1]), sm('lg', [128, 36]), sm('gmax', [128, 2]), sm('goh', [128, 4]), \
                sm('ex', [128, 4]), sm('gsum', [128, 2]), sm('sel', [128, 8])
            m12, oh1, oh2, sel2, pp, Af = sm('m12', [128, 4]), sm('oh1', [128, 8]), sm('oh2', [128, 8]), \
                sm('sel2', [128, 8]), sm('pp', [128, 4]), sm('Af', [128, 32])

            def tile_body(i):
                q = i % NB
                x_, hT_, sq_ = xt[q], h2T[q], sq[q]
                bank = ps[q]
                r0 = i * 128
                H.dma(x_[:], Sx.x1[r0:r0 + 128, :], [('x1', r0)], [x_])
                H.memset('pool', ss[q][:], 0.0, [ss[q]])
                P.act(lambda e, o=sq_[:], a=x_[:], acc=ss[q][:]: e.activation(o, a, AF.Square, accum_out=acc), [x_, ss[q]], [sq_, ss[q]])
                H.rsqrt(ss[q][:], ss[q][:], 1.0 / D, EPS, [ss[q]], [ss[q]])
                H.ts('dve', x_[:], x_[:], ss[q][:, 0:1], None, ALU.mult, None, [x_, ss[q]], [x_])
                H.tt('dve', hb_all[:, i, :], x_[:], gfrow[:], ALU.mult, [x_, gfrow], [(hb_all, i)])
                for hf in range(2):
                    for k4 in range(4):
                        kt = hf * 4 + k4
                        H.tr(bank[:, k4 * 128:(k4 + 1) * 128], x_[:, kt * 128:(kt + 1) * 128], identf[:], [x_, identf], [bank])
                    H.cp('act', hT_[:, hf * 4:(hf + 1) * 4, :], bank[:].rearrange("p (a c) -> p a c", a=4), [bank], [(hT_, hf)])
                for kt in range(8):
                    H.mm(bank[:, 0:36], hT_[:, kt, :], RW[:, kt, :], kt == 0, kt == 7, [(hT_, kt // 4), RW], [bank])
                L = lg[q]
                H.tt('dve', L[:], bank[:, 0:36], rbias[:], ALU.add, [bank, rbias], [L])
                gm, go, e_, gs, se = gmax[q], goh[q], ex[q], gsum[q], sel[q]
                H.red('dve', gm[:, 0:1], L[:, 0:4], ALU.max, [L], [gm])
                H.ts('dve', go[:], L[:, 0:4], gm[:, 0:1], None, ALU.is_equal, None, [L, gm], [go])
                H.ts('dve', gm[:, 1:2], gm[:, 0:1], -1.0, None, ALU.mult, None, [gm], [gm])
                H.actf(e_[:], L[:, 0:4], AF.Exp, [L, gm], [e_], bias=gm[:, 1:2])
                H.red('dve', gs[:, 0:1], e_[:], ALU.add, [e_], [gs])
                H.recip(gs[:, 1:2], gs[:, 0:1], [gs], [gs])
                H.ts('dve', se[:], L[:, 4:12], go[:, 0:1], None, ALU.mult, None, [L, go], [se])
                for g in range(1, 4):
                    H.stt('dve', se[:], L[:, 4 + g * 8:12 + g * 8], go[:, g:g + 1], se[:], ALU.mult, ALU.add, [L, go, se], [se])
                mm_, o1_, o2_, s2_, p_ = m12[q], oh1[q], oh2[q], sel2[q], pp[q]
                H.red('dve', mm_[:, 0:1], se[:], ALU.max, [se], [mm_])
                H.ts('dve', o1_[:], se[:], mm_[:, 0:1], None, ALU.is_equal, None, [se, mm_], [o1_])
                H.stt('dve', s2_[:], o1_[:], -1e30, se[:], ALU.mult, ALU.add, [o1_, se], [s2_])
                H.red('dve', mm_[:, 1:2], s2_[:], ALU.max, [s2_], [mm_])
                H.ts('dve', o2_[:], s2_[:], mm_[:, 1:2], None, ALU.is_equal, None, [s2_, mm_], [o2_])
                H.tt('dve', mm_[:, 2:3], mm_[:, 1:2], mm_[:, 0:1], ALU.subtract, [mm_], [mm_])
                H.actf(p_[:, 0:1], mm_[:, 2:3], AF.Exp, [mm_], [p_])
                H.ts('dve', p_[:, 1:2], p_[:, 0:1], 1.0, None, ALU.add, None, [p_], [p_])
                H.recip(p_[:, 2:3], p_[:, 1:2], [p_], [p_])
                H.tt('dve', p_[:, 3:4], p_[:, 0:1], p_[:, 2:3], ALU.mult, [p_], [p_])
                H.tt('dve', cw[0][:, i:i + 1], p_[:, 2:3], gs[:, 1:2], ALU.mult, [p_, gs], [(cw[0], i)])
                H.tt('dve', cw[1][:, i:i + 1], p_[:, 3:4], gs[:, 1:2], ALU.mult, [p_, gs], [(cw[1], i)])
                for k, ok in enumerate((o1_, o2_)):
                    for g in range(4):
                        H.ts('pool', oh_all[:, k, i, g * 8:(g + 1) * 8], ok[:], go[:, g:g + 1], None, ALU.mult, None, [ok, go], [(oh_all, i)])
                H.tt('pool', Af[q][:], oh_all[:, 0, i, :], oh_all[:, 1, i, :], ALU.add, [(oh_all, i)], [Af[q]])
                H.cp('pool', Abf_all[:, i, :], Af[q][:], [Af[q]], [(Abf_all, i)])
                o = bank[:, 64:96]
                H.mm(o, tris[:], Abf_all[:, i, :], True, i == 0, [tris, (Abf_all, i)], [bank])
                for j in range(i):
                    H.mm(o, ones[:], Abf_all[:, j, :], False, j == i - 1, [ones, (Abf_all, j)], [bank])
                pz = pos_all[:, i, :]
                H.ts('dve', pz, o, float(CAP - 1), None, ALU.min, None, [bank], [(pos_all, i)])
                H.tt('dve', pz, pz, ecap[:], ALU.add, [(pos_all, i), ecap], [(pos_all, i)])
                for k in range(2):
                    H.tt('dve', prod_all[:, i, :], oh_all[:, k, i, :], pz, ALU.mult, [(oh_all, i), (pos_all, i)], [(prod_all, i)])
                    H.red('dve', slf_all[:, k, i:i + 1], prod_all[:, i, :], ALU.add, [(prod_all, i)], [(slf_all, k, i)])
                    H.cp('dve', slots[k][:, i:i + 1], slf_all[:, k, i:i + 1], [(slf_all, k, i)], [(slots[k], i)])
                for k in range(2):
                    P.dma(lambda e, k=k, i=i: e.indirect_dma_start(
                        out=Sx.xg.ap(), out_offset=IOA(ap=slots[k][:, i:i + 1], axis=0), in_=hb_all[:, i, :], in_offset=None),
                        [(hb_all, i), (slots[k], i)], ['xg'], q='pool')

            def capture(fn):
                ops = []
                P.add = lambda E, f, r=(), w=(), dma=False: ops.append((E, f, r, w, dma))
                try:
                    fn()
                finally:
                    del P.add
                return ops

            active = []
            nxt = 0
            step = 0
            K_ = None
            while active or nxt < NT:
                if nxt < NT and len(active) < NB and (K_ is None or step % K_ == 0):
                    ops_ = capture(lambda i=nxt: tile_body(i))
                    if K_ is None:
                        K_ = max(1, -(-len(ops_) // NB))
                    active.append([ops_, 0])
                    nxt += 1
                for ch in list(active):
                    P.add(*ch[0][ch[1]])
                    ch[1] += 1
                    if ch[1] >= len(ch[0]):
                        active.remove(ch)
                step += 1
        P.barrier()
        with ExitStack() as s2:
            stg1 = [sbt(G, s2, 'f_stg1%d' % i, [128, 8, 256], F32) for i in range(2)]
            stg3 = [sbt(G, s2, 'f_stg3%d' % i, [128, 8, 256], F32) for i in range(2)]
            stg2 = [sbt(G, s2, 'f_stg2%d' % i, [128, 2, 1024], F32) for i in range(2)]
            W1 = [sbt(G, s2, 'f_W1%d' % i, [128, 8, 256], BF16) for i in range(2)]
            W3 = [sbt(G, s2, 'f_W3%d' % i, [128, 8, 256], BF16) for i in range(2)]
            W2 = [sbt(G, s2, 'f_W2%d' % i, [128, 2, 1024], BF16) for i in range(2)]
            xg = [sbt(G, s2, 'f_xg%d' % i, [128, NSLOT, 1024], BF16) for i in range(2)]
            xgT = [sbt(G, s2, 'f_xgT%d' % i, [128, 8, CAP], BF16) for i in range(2)]
            sa = [sbt(G, s2, 'f_sa%d' % i, [128, CAP], F32) for i in range(2)]
            hid = [sbt(G, s2, 'f_hid%d' % i, [128, 2, CAP], BF16) for i in range(2)]
            yo = [sbt(G, s2, 'f_yo%d' % i, [128, 1024], BF16) for i in range(3)]
            yi = 0
            NE = 32 if 'noF2' not in G.dbg else 0

            def prefetch(e):
                q = e % 2
                H.dma(stg1[q][:], I.w1[e].rearrange("(p k) c -> p k c", k=8), [], [stg1[q]])
                H.dma(stg3[q][:], I.w3[e].rearrange("(p k) c -> p k c", k=8), [], [stg3[q]])
                H.dma(stg2[q][:], I.w2[e].rearrange("(k p) c -> p k c", p=128), [], [stg2[q]])
                H.dma(xg[q][:], Sx.xg[e * CAP:(e + 1) * CAP, :].rearrange("(a p) c -> p a c", p=128), ['xg'], [xg[q]])
                H.cp('dve', W1[q][:], stg1[q][:], [stg1[q]], [W1[q]])
                H.cp('act', W3[q][:], stg3[q][:], [stg3[q]], [W3[q]])
                H.cp('act', W2[q][:], stg2[q][:], [stg2[q]], [W2[q]])

            if NE:
                prefetch(0)
            for e in range(NE):
                q = e % 2
                if e + 1 < NE:
                    prefetch(e + 1)
                for a in range(NSLOT):
                    pb_ = pbk[a % 2]
                    for kt in range(8):
                        H.tr(pb_[:, kt * 128:(kt + 1) * 128], xg[q][:, a, kt:1024:8], ident[:], [xg[q], ident], [pb_])
                    H.cp('act' if a % 2 == 0 else 'dve', xgT[q][:, :, a * 128:(a + 1) * 128], pb_[:].rearrange("p (k t) -> p k t", k=8),
                         [pb_], [(xgT[q], a)])
                xall = [(xgT[q], a) for a in range(NSLOT)]
                for j in range(2):
                    pa, pb2 = ps[0 + 2 * j], ps[1 + 2 * j]
                    for kt in range(8):
                        H.mm(pa[:, 0:CAP], W1[q][:, kt, j * 128:(j + 1) * 128], xgT[q][:, kt, :], kt == 0, kt == 7, [W1[q]] + xall, [pa])
                    for kt in range(8):
                        H.mm(pb2[:, 0:CAP], W3[q][:, kt, j * 128:(j + 1) * 128], xgT[q][:, kt, :], kt == 0, kt == 7, [W3[q]] + xall, [pb2])
                    H.actf(sa[j][:], pa[:, 0:CAP], AF.Silu, [pa], [sa[j]])
                    H.tt('dve', hid[q][:, j, :], sa[j][:], pb2[:, 0:CAP], ALU.mult, [sa[j], pb2], [(hid[q], j)])
                for a in range(NSLOT):
                    y_ = yo[yi % 3]
                    yi += 1
                    for half in range(2):
                        po = ps[4 + half]
                        for j in range(2):
                            H.mm(po[:], hid[q][:, j, a * 128:(a + 1) * 128], W2[q][:, j, half * 512:(half + 1) * 512], j == 0, j == 1,
                                 [(hid[q], j), W2[q]], [po])
                        H.cp('act' if half == 0 else 'dve', y_[:, half * 512:(half + 1) * 512], po[:], [po], [(y_, half)])
                    r0 = e * CAP + a * 128
                    H.dma(Sx.yg[r0:r0 + 128, :], y_[:], [(y_, 0), (y_, 1)], ['yg'], q='pool')
        P.barrier()
        with ExitStack() as s3:
            gfin = sbt(G, s3, 'f_gfin', [128, D], F32)
            H.dma(gfin[:], I.gfin_b.ap(), [], [gfin])
            NB3 = 4
            r1 = [sbt(G, s3, 'f_r1%d' % i, [128, D], BF16) for i in range(NB3)]
            r2 = [sbt(G, s3, 'f_r2%d' % i, [128, D], BF16) for i in range(NB3)]
            xt = [sbt(G, s3, 'f_x3%d' % i, [128, D], F32) for i in range(NB3)]
            sq = [sbt(G, s3, 'f_sq3%d' % i, [128, D], BF16) for i in range(NB3)]
            ss = [sbt(G, s3, 'f_ss3%d' % i, [128, 1], F32) for i in range(NB3)]

            def f3_body(i):
                q = i % NB3
                r0 = i * 128
                H.dma(xt[q][:], Sx.x1[r0:r0 + 128, :], [('x1', r0)], [xt[q]])
                for k, rr_ in enumerate((r1[q], r2[q])):
                    P.dma(lambda e, k=k, i=i, rr_=rr_: e.indirect_dma_start(
                        out=rr_[:], out_offset=None, in_=Sx.yg.ap(), in_offset=IOA(ap=slots[k][:, i:i + 1], axis=0)),
                        ['yg', (slots[k], i)], [rr_], q='pool')
                H.memset('pool', ss[q][:], 0.0, [ss[q]])
                H.stt('dve', xt[q][:], r1[q][:], cw[0][:, i:i + 1], xt[q][:], ALU.mult, ALU.add, [r1[q], (cw[0], i), xt[q]], [xt[q]])
                H.stt('dve', xt[q][:], r2[q][:], cw[1][:, i:i + 1], xt[q][:], ALU.mult, ALU.add, [r2[q], (cw[1], i), xt[q]], [xt[q]])
                P.act(lambda e, o=sq[q][:], a=xt[q][:], acc=ss[q][:]: e.activation(o, a, AF.Square, accum_out=acc), [xt[q], ss[q]], [sq[q], ss[q]])
                H.rsqrt(ss[q][:], ss[q][:], 1.0 / D, EPS, [ss[q]], [ss[q]])
                H.stt('dve', xt[q][:], xt[q][:], ss[q][:, 0:1], gfin[:], ALU.mult, ALU.mult, [xt[q], ss[q], gfin], [xt[q]])
                H.dma(G.out[r0:r0 + 128, :], xt[q][:], [xt[q]], [('out', i)])

            def capture_f3(fn):
                ops = []
                P.add = lambda E, f, r=(), w=(), dma=False: ops.append((E, f, r, w, dma))
                try:
                    fn()
                finally:
                    del P.add
                return ops

            NT3 = NT if 'noF3' not in G.dbg else 0
            active = []
            nxt = 0
            step = 0
            while active or nxt < NT3:
                if nxt < NT3 and len(active) < NB3 and step % 3 == 0:
                    active.append([capture_f3(lambda i=nxt: f3_body(i)), 0])
                    nxt += 1
                for ch in list(active):
                    P.add(*ch[0][ch[1]])
                    ch[1] += 1
                    if ch[1] >= len(ch[0]):
                        active.remove(ch)
                step += 1


def host_prep(inp):
    f32 = np.float32
    bf = ml_dtypes.bfloat16
    out = {}
    w_in = np.asarray(inp['w_in'][0], f32)
    ext = np.zeros((D, NW), f32)
    ext[:, :4640] = w_in
    kr = w_in[:, 640:672]
    ext[:, KA_OFF + 64:KA_OFF + 96] = kr
    ext[:, KB_OFF + 64:KB_OFF + 80] = kr[:, 16:32]
    ext[:, KB_OFF + 80:KB_OFF + 96] = kr[:, 0:16]
    out['w_in_ext'] = ext
    out['g_mix'] = np.ascontiguousarray(np.asarray(inp['norm_mix_g'][0], f32).reshape(8, 128).T)
    wuq = np.asarray(inp['w_uq'][0], f32).reshape(384, 8, 96)
    e = np.zeros((384, 2, 8, 128), f32)
    e[:, 0, :, 0:96] = wuq
    e[:, 1, :, 64:80] = wuq[:, :, 80:96]
    e[:, 1, :, 80:96] = wuq[:, :, 64:80]
    out['wuq_ext'] = e.reshape(384, 2048)
    out['g_q'] = np.ascontiguousarray(np.asarray(inp['q_norm_g'][0], f32).reshape(3, 128).T)
    wukv = np.asarray(inp['w_ukv'][0], f32).reshape(256, 8, 128)
    e = np.zeros((256, 2, 8, 128), f32)
    e[:, 0, :, 0:64] = wukv[:, :, 0:64]
    for h in range(8):
        o = (h % 2) * 64
        e[:, 1, h, o:o + 64] = wukv[:, h, 64:128]
    out['wukv_ext'] = e.reshape(256, 2048)
    out['g_kv'] = np.ascontiguousarray(np.asarray(inp['kv_norm_g'][0], f32).reshape(2, 128).T)
    pos = np.arange(S, dtype=np.float32)
    inv_freq = (10000.0 ** (-np.arange(0, 32, 2, dtype=np.float32) / 32)).astype(np.float32)
    ang = pos[None, :] * inv_freq[:, None]
    cos, sin = np.cos(ang).astype(f32), np.sin(ang).astype(f32)
    c128 = np.ones((128, S), f32)
    s128 = np.zeros((128, S), f32)
    c128[64:80] = cos
    c128[80:96] = cos
    s128[64:80] = -sin
    s128[80:96] = sin
    out['cos128'] = c128
    out['sin128'] = s128
    out['ident_bf'] = np.eye(128, dtype=f32).astype(bf)
    out['ones_bf'] = np.ones((128, 128), f32).astype(bf)
    op = np.zeros((2, 128, 128), f32)
    op[0, :, 0:64] = 1
    op[1, :, 64:128] = 1
    out['onespad_bf'] = op.astype(bf)
    def cols(v, n):
        return np.asarray(v, f32).reshape(n, 128).T
    cst = np.zeros((128, 64), f32)
    cst[:, 0:15] = cols(inp['mu_prev'][0], 15)
    cst[:, 15:30] = cols(inp['mu_next'][0], 15)
    cst[:, 30:34] = cols(inp['k_k'][0], 4)
    cst[:, 34:38] = cols(inp['k_a'][0], 4)
    cst[:, 38:42] = cols(inp['r_k'][0], 4)
    cst[:, 42:46] = cols(inp['lnx_g'][0], 4)
    cst[:, 46:50] = cols(inp['lnx_b'][0], 4)
    a0 = np.asarray(inp['a0'][0], f32)
    cst[:, 50:54] = cols(a0[0], 4)
    cst[:, 54:58] = cols(a0[1], 4)
    out['rw_cst'] = cst
    out['w_up2'] = np.ascontiguousarray(np.asarray(inp['w_up'][0], f32).reshape(128, 512))
    out['a_up2'] = np.ascontiguousarray(np.asarray(inp['a_up'][0], f32).reshape(128, 512))
    out['g_up2'] = np.ascontiguousarray(np.asarray(inp['g_up'][0], f32))
    w0 = np.asarray(inp['w0'][0], f32)
    out['w0b'] = np.ascontiguousarray(np.broadcast_to(w0[:, None, :], (2, 128, 512)))
    idx = np.arange(128)
    same = (idx[:, None] // 64) == (idx[None, :] // 64)
    sp, tp = idx[:, None] % 64, idx[None, :] % 64
    cf = -np.exp(-0.5)
    tri3 = np.zeros((2, 128, 384), f32)
    tri3[0, :, 0:128] = same & (sp <= tp)
    tri3[0, :, 128:256] = same & (sp < tp)
    tri3[0, :, 256:384] = same & (sp > tp)
    tri3[1, :, 0:128] = same & (sp >= tp)
    tri3[1, :, 128:256] = same & (sp > tp)
    tri3[1, :, 256:384] = same & (sp < tp)
    out['tri3'] = (tri3 * cf).astype(f32)
    mT = np.zeros((2, 128, 2, 256), f32)
    mL = np.zeros((2, 128, 4, 128), f32)
    for a in range(2):
        mT[0, :, a, 0:128] = same & (tp > sp)
        mT[0, :, a, 128:256] = same & (tp >= sp)
        mT[1, :, a, 0:128] = same & (tp < sp)
        mT[1, :, a, 128:256] = same & (tp <= sp)
    for a in range(4):
        mL[0, :, a, :] = same & (tp < sp)
        mL[1, :, a, :] = same & (tp > sp)
    out['maskT'] = mT.reshape(2, 128, 512).astype(bf)
    out['maskL'] = mL.reshape(2, 128, 512).astype(bf)
    out['ident4'] = np.tile(np.eye(128, dtype=f32), (1, 4)).astype(bf)
    out['bdones_bf'] = same.astype(f32).astype(bf)
    out['bdones_f'] = same.astype(f32)
    out['w_br_mla'] = np.ascontiguousarray(np.asarray(inp['w_br_mla'][0], f32))
    out['w_br_rwkv'] = np.ascontiguousarray(np.asarray(inp['w_br_rwkv'][0], f32))
    out['w_out'] = np.ascontiguousarray(np.asarray(inp['w_out'][0], f32))
    gbv = np.asarray(inp['gate_b'][0], f32)
    out['gate_b2'] = np.ascontiguousarray(gbv.reshape(16, 128).T)
    gf = cols(inp['norm_ffn_g'][0], 8)
    out['gffn_b'] = np.ascontiguousarray(np.broadcast_to(gf[:, :, None], (128, 8, 256)))
    out['gffn_row_b'] = np.ascontiguousarray(np.broadcast_to(np.asarray(inp['norm_ffn_g'][0], f32)[None, :], (128, 1024)))
    out['ident_f'] = np.eye(128, dtype=f32)
    out['tris_bf'] = (idx[:, None] < idx[None, :]).astype(f32).astype(bf)
    rwc = np.concatenate([np.asarray(inp['router_group_w'][0], f32), np.asarray(inp['router_expert_w'][0], f32)], axis=1)
    out['rw_router'] = np.ascontiguousarray(rwc.reshape(8, 128, 36).transpose(1, 0, 2))
    rb = np.concatenate([np.asarray(inp['router_group_b'][0], f32), np.asarray(inp['router_expert_b'][0], f32)])
    out['rbias_b'] = np.ascontiguousarray(np.broadcast_to(rb[None, :], (128, 36)))
    out['ecap_b'] = np.ascontiguousarray(np.broadcast_to((np.arange(32, dtype=f32) * CAP)[None, :], (128, 32)))
    out['ecap_all'] = np.ascontiguousarray(np.broadcast_to((np.arange(32, dtype=f32) * CAP)[None, None, :], (128, 32, 32)))
    out['w1'] = np.ascontiguousarray(np.asarray(inp['w1'][0], f32))
    out['w3'] = np.ascontiguousarray(np.asarray(inp['w3'][0], f32))
    out['w2'] = np.ascontiguousarray(np.asarray(inp['w2'][0], f32))
    out['gfin_b'] = np.ascontiguousarray(np.broadcast_to(np.asarray(inp['norm_final_g'], f32)[None, :], (128, 1024)))
    return out


_NC_CACHE = {}


def kernel(**inputs):
    n = 8
    hp = host_prep(inputs)
    x = np.ascontiguousarray(np.asarray(inputs['x'], np.float32)).reshape(16 * S, D)
    if 'nc' not in _NC_CACHE:
        _NC_CACHE['nc'] = build_nc(nseq=NSEQ_CORE)
    nc = _NC_CACHE['nc']
    rows = NSEQ_CORE * S
    in_maps = []
    for c in range(n):
        m = dict(hp)
        m['x'] = np.ascontiguousarray(x[c * rows:(c + 1) * rows])
        in_maps.append(m)
    res = run_bass_kernel_spmd(nc, in_maps, core_ids=list(range(n)))
    outs = [np.asarray(r['out'], np.float32) for r in res.results]
    return np.concatenate(outs, axis=0).reshape(16, S, D)
```
